# Optimizing a Trainium2 kernel written in Bass

```python
import math
import jax
import jax.numpy as jnp
from jax import lax
import numpy as np

D_MODEL = 1024
BATCH = 8
SEQ = 2048
DEPTH = 2

GRID_W = 64
CTX_LEN = 256
HEAD_DIM = 64
DIFF_HEADS = 4
DIFF_V_DIM = 2 * HEAD_DIM
WIN_Q_HEADS = 8
WIN_KV_HEADS = 2
WINDOW = 128
WIN_BLOCK = 128
Q_BLOCK = 128
ROPE_BASE = 10000.0
DIFF_QK_W = DIFF_HEADS * 2 * HEAD_DIM
DIFF_VW = DIFF_HEADS * DIFF_V_DIM
WIN_Q_W = WIN_Q_HEADS * HEAD_DIM
WIN_KV_W = WIN_KV_HEADS * HEAD_DIM
Q_W = DIFF_QK_W + WIN_Q_W
KV_W = DIFF_QK_W + DIFF_VW + 2 * WIN_KV_W
ATTN_PROJ_W = Q_W + KV_W
MIX_W = DIFF_VW + WIN_Q_W
HYENA_ORDER = 2
SHORT_CONV = 3
FILTER_EMB = 33
FILTER_HID = 64
FILTER_OUT_SCALE = 0.05
DECAY_TARGET = 1e-2
FAST_DECAY_PCT = 0.3
SLOW_DECAY_PCT = 1.5
N_EXPERTS = 16
N_GROUPS = 4
EXPERTS_PER_GROUP = N_EXPERTS // N_GROUPS
TOP_K = 2
EXPERT_FF = 1024
MOE_BLOCK = 128
LN_EPS = 1e-5
DEEPNORM_ALPHA = (2 * DEPTH) ** 0.25
DEEPNORM_BETA = (8 * DEPTH) ** -0.25
NEG_INF = -1e30

kernel_name = 'hybrid_diffattn_wingqa_hyena_groupmoe'


def layer_norm(x, g, b):
    xf = x.astype(jnp.float32)
    mu = xf.mean(-1, keepdims=True)
    var = jnp.square(xf - mu).mean(-1, keepdims=True)
    y = (xf - mu) * lax.rsqrt(var + LN_EPS) * g.astype(jnp.float32) + b.astype(jnp.float32)
    return y.astype(x.dtype)


def rms_norm(x, g):
    xf = x.astype(jnp.float32)
    y = xf * lax.rsqrt(jnp.mean(jnp.square(xf), -1, keepdims=True) + LN_EPS) * g.astype(jnp.float32)
    return y.astype(x.dtype)


def axial_rope_tables(rows):
    r, col = jnp.meshgrid(jnp.arange(rows, dtype=jnp.float32), jnp.arange(GRID_W, dtype=jnp.float32), indexing='ij')
    axis_dim = HEAD_DIM // 2
    inv_freq = ROPE_BASE ** (-jnp.arange(0, axis_dim, 2, dtype=jnp.float32) / axis_dim)
    ang = jnp.concatenate([r.reshape(-1, 1) * inv_freq, col.reshape(-1, 1) * inv_freq], -1)
    ang = jnp.concatenate([ang, ang], -1)
    return jnp.cos(ang), jnp.sin(ang)


def apply_rope(x, cos, sin):
    shape = (1, cos.shape[0]) + (1,) * (x.ndim - 3) + (HEAD_DIM,)
    cos = cos.reshape(shape).astype(x.dtype)
    sin = sin.reshape(shape).astype(x.dtype)
    x1, x2 = jnp.split(x, 2, axis=-1)
    return x * cos + jnp.concatenate([-x2, x1], -1) * sin


def diff_attention(q, k, v, lam):
    b, n, h, _, d = q.shape
    nb = n // Q_BLOCK
    scale = d ** -0.5
    qb = jnp.moveaxis(q.reshape(b, nb, Q_BLOCK, h, 2, d), 1, 0)

    def block(q_blk):
        s = jnp.einsum('bqhmd,bkhmd->mbhqk', q_blk, k).astype(jnp.float32) * scale
        p = jax.nn.softmax(s, axis=-1)
        a = (p[0] - lam * p[1]).astype(v.dtype)
        return jnp.einsum('bhqk,bkhe->bqhe', a, v)

    o = lax.map(block, qb)
    return jnp.moveaxis(o, 0, 1).reshape(b, n, h, v.shape[-1])


def window_gqa_latent(q, k, v, k_ctx, v_ctx, sink):
    b, n, hq, d = q.shape
    nb = n // WIN_BLOCK
    g = hq // WIN_KV_HEADS
    scale = d ** -0.5
    qb = q.reshape(b, nb, WIN_BLOCK, WIN_KV_HEADS, g, d)

    def band(t):
        tp = jnp.pad(t, ((0, 0), (WIN_BLOCK, WIN_BLOCK), (0, 0), (0, 0))).reshape(b, nb + 2, WIN_BLOCK, WIN_KV_HEADS, d)
        return jnp.concatenate([tp[:, :-2], tp[:, 1:-1], tp[:, 2:]], axis=2)

    kb, vb = band(k), band(v)
    n_loc = 3 * WIN_BLOCK
    s_loc = jnp.einsum('bnqhgd,bnkhd->bnhgqk', qb, kb).astype(jnp.float32) * scale
    q_off = jnp.arange(WIN_BLOCK)[:, None] + WIN_BLOCK
    k_off = jnp.arange(n_loc)[None, :]
    k_abs = (jnp.arange(nb)[:, None] - 1) * WIN_BLOCK + k_off
    allowed = (jnp.abs(q_off - k_off) <= WINDOW)[None] & ((k_abs >= 0) & (k_abs < n))[:, None, :]
    s_loc = jnp.where(allowed[None, :, None, None], s_loc, NEG_INF)
    s_ctx = jnp.einsum('bnqhgd,bchd->bnhgqc', qb, k_ctx).astype(jnp.float32) * scale
    s_sink = jnp.broadcast_to(sink.astype(jnp.float32).reshape(WIN_KV_HEADS, g)[None, None, :, :, None, None], s_loc.shape[:-1] + (1,))
    p = jax.nn.softmax(jnp.concatenate([s_loc, s_ctx, s_sink], -1), axis=-1).astype(v.dtype)
    n_ctx = k_ctx.shape[1]
    o = jnp.einsum('bnhgqk,bnkhd->bnqhgd', p[..., :n_loc], vb) + jnp.einsum('bnhgqc,bchd->bnqhgd', p[..., n_loc:n_loc + n_ctx], v_ctx)
    return o.reshape(b, n, hq, d)


def gqa_sink_dense(q, k, v, sink):
    b, n, hq, d = q.shape
    g = hq // WIN_KV_HEADS
    qg = q.reshape(b, n, WIN_KV_HEADS, g, d)
    s = jnp.einsum('bqhgd,bkhd->bhgqk', qg, k).astype(jnp.float32) * d ** -0.5
    s_sink = jnp.broadcast_to(sink.astype(jnp.float32).reshape(WIN_KV_HEADS, g)[None, :, :, None, None], s.shape[:-1] + (1,))
    p = jax.nn.softmax(jnp.concatenate([s, s_sink], -1), axis=-1)[..., :-1].astype(v.dtype)
    return jnp.einsum('bhgqk,bkhd->bqhgd', p, v).reshape(b, n, hq, d)


def attention_group_mixer(h, h_ctx, w_in, lam_vec, subln_g, sink, w_out, lam_init, cos, sin, ctx_out):
    def split_q(cols):
        qa = cols[..., :DIFF_QK_W].reshape(cols.shape[:2] + (DIFF_HEADS, 2, HEAD_DIM))
        qw = cols[..., DIFF_QK_W:Q_W].reshape(cols.shape[:2] + (WIN_Q_HEADS, HEAD_DIM))
        return qa, qw

    def split_kv(cols):
        o1 = DIFF_QK_W
        o2 = o1 + DIFF_VW
        o3 = o2 + WIN_KV_W
        ka = cols[..., :o1].reshape(cols.shape[:2] + (DIFF_HEADS, 2, HEAD_DIM))
        va = cols[..., o1:o2].reshape(cols.shape[:2] + (DIFF_HEADS, DIFF_V_DIM))
        kw = cols[..., o2:o3].reshape(cols.shape[:2] + (WIN_KV_HEADS, HEAD_DIM))
        vw = cols[..., o3:].reshape(cols.shape[:2] + (WIN_KV_HEADS, HEAD_DIM))
        return ka, va, kw, vw

    lv = lam_vec.astype(jnp.float32)
    lam = jnp.exp(jnp.sum(lv[0] * lv[1])) - jnp.exp(jnp.sum(lv[2] * lv[3])) + lam_init

    def merge(oa, ow):
        oa = rms_norm(oa, subln_g) * (1.0 - lam_init)
        cat = jnp.concatenate([oa.reshape(oa.shape[:2] + (DIFF_VW,)), ow.reshape(ow.shape[:2] + (WIN_Q_W,))], -1)
        return cat @ w_out

    proj = h @ w_in
    proj_c = h_ctx @ (w_in if ctx_out else w_in[:, Q_W:])
    ka_c, va_c, kw_c, vw_c = split_kv(proj_c[..., -KV_W:])
    qa, qw = split_q(proj[..., :Q_W])
    ka, va, kw, vw = split_kv(proj[..., Q_W:])
    qa, ka, qw, kw = apply_rope(qa, cos, sin), apply_rope(ka, cos, sin), apply_rope(qw, cos, sin), apply_rope(kw, cos, sin)
    oa = diff_attention(qa, jnp.concatenate([ka, ka_c], 1), jnp.concatenate([va, va_c], 1), lam)
    ow = window_gqa_latent(qw, kw, vw, kw_c, vw_c, sink)
    y = merge(oa, ow)
    if ctx_out:
        qa_c, qw_c = split_q(proj_c[..., :Q_W])
        y_c = merge(diff_attention(qa_c, ka_c, va_c, lam), gqa_sink_dense(qw_c, kw_c, vw_c, sink))
        return y, y_c
    return y, None


def hyena_filter_spectra(n, ffn_w_in, ffn_w_hid, ffn_b, sin_freq, ffn_w_out):
    f32 = jnp.float32
    t = jnp.linspace(0.0, 1.0, n, dtype=f32)[:, None]
    bands = (FILTER_EMB - 1) // 2
    w = 2.0 * math.pi * jnp.arange(n, dtype=f32)[:, None] / n
    fr = jnp.linspace(1e-4, bands - 1, bands, dtype=f32)[None, :]
    z = jnp.concatenate([t, jnp.cos(fr * w), -jnp.sin(fr * w)], -1)
    sf, fb = sin_freq.astype(f32), ffn_b.astype(f32)
    hid = jnp.sin(sf[0] * (z @ ffn_w_in.astype(f32) + fb[0]))
    hid = jnp.sin(sf[1] * (hid @ ffn_w_hid[0].astype(f32) + fb[1]))
    hid = jnp.sin(sf[2] * (hid @ ffn_w_hid[1].astype(f32) + fb[2]))
    filt = (hid @ ffn_w_out.astype(f32)).reshape(n, 2, HYENA_ORDER, D_MODEL)
    deltas = jnp.abs(jnp.linspace(math.log(DECAY_TARGET) / SLOW_DECAY_PCT, math.log(DECAY_TARGET) / FAST_DECAY_PCT, D_MODEL, dtype=f32))
    filt = filt * jnp.exp(-t * deltas)[:, None, None, :]
    fwd, bwd = filt[:, 0], filt[:, 1]
    kern = jnp.concatenate([fwd, jnp.zeros_like(fwd[:1]), jnp.flip(bwd[1:], 0)], 0)
    return jnp.fft.rfft(kern, axis=0)


def hyena_mixer(h, w_in, conv_w, conv_b, ffn_w_in, ffn_w_hid, ffn_b, sin_freq, ffn_w_out, skip, w_out):
    n = h.shape[1]
    u = h @ w_in
    up = jnp.pad(u, ((0, 0), (1, 1), (0, 0)))
    u = up[:, :-2] * conv_w[0] + up[:, 1:-1] * conv_w[1] + up[:, 2:] * conv_w[2] + conv_b
    v, x1, x2 = jnp.split(u, 3, axis=-1)
    spec = hyena_filter_spectra(n, ffn_w_in, ffn_w_hid, ffn_b, sin_freq, ffn_w_out)
    sk = skip.astype(jnp.float32)
    z = v
    for o, gate in enumerate((x1, x2)):
        zf32 = z.astype(jnp.float32)
        conv = jnp.fft.irfft(jnp.fft.rfft(zf32, n=2 * n, axis=1) * spec[None, :, o], n=2 * n, axis=1)[:, :n]
        z = gate * (conv + zf32 * sk[o]).astype(h.dtype)
    return z @ w_out


def moe_ffn(h, router_w, router_bias, w_gate, w_up, w_down):
    b, n, d = h.shape
    t_count = b * n
    hf = h.reshape(t_count, d)
    scores = jax.nn.softmax((hf @ router_w).astype(jnp.float32), axis=-1)
    sel = (scores + router_bias.astype(jnp.float32)).reshape(t_count, N_GROUPS, EXPERTS_PER_GROUP)
    group_score = lax.top_k(sel, 2)[0].sum(-1)
    grp = jnp.argmax(group_score, axis=-1)
    in_group = jnp.take_along_axis(sel, grp[:, None, None], axis=1)[:, 0]
    _, local = lax.top_k(in_group, TOP_K)
    e_idx = grp[:, None] * EXPERTS_PER_GROUP + local
    wts = jnp.take_along_axis(scores, e_idx, axis=-1)
    wts = wts / wts.sum(-1, keepdims=True)
    n_assign = t_count * TOP_K
    flat_e = e_idx.reshape(n_assign)
    flat_tok = jnp.arange(n_assign, dtype=jnp.int32) // TOP_K
    order = jnp.argsort(flat_e)
    se = flat_e[order]
    counts = jnp.bincount(flat_e, length=N_EXPERTS)
    pcounts = (counts + MOE_BLOCK - 1) // MOE_BLOCK * MOE_BLOCK
    pends = jnp.cumsum(pcounts)
    pstarts = pends - pcounts
    starts = jnp.cumsum(counts) - counts
    dest = pstarts[se] + jnp.arange(n_assign) - starts[se]
    n_blk = -(-(n_assign + N_EXPERTS * (MOE_BLOCK - 1)) // MOE_BLOCK)
    slot_tok = jnp.full((n_blk * MOE_BLOCK,), t_count, jnp.int32).at[dest].set(flat_tok[order])
    slot_w = jnp.zeros((n_blk * MOE_BLOCK,), jnp.float32).at[dest].set(wts.reshape(n_assign)[order])
    blk_e = jnp.minimum(jnp.searchsorted(pends, jnp.arange(n_blk) * MOE_BLOCK, side='right'), N_EXPERTS - 1)
    xs = jnp.concatenate([hf, jnp.zeros((1, d), hf.dtype)], 0)[slot_tok].reshape(n_blk, MOE_BLOCK, d)

    def expert_block(args):
        xb, e = args
        return (jax.nn.silu(xb @ w_gate[e]) * (xb @ w_up[e])) @ w_down[e]

    ys = lax.map(expert_block, (xs, blk_e)).reshape(n_blk * MOE_BLOCK, d)
    out = jnp.zeros((t_count + 1, d), hf.dtype).at[slot_tok].add(ys * slot_w[:, None].astype(ys.dtype))
    return out[:t_count].reshape(b, n, d)


def setup_inputs(seed: int = 0) -> dict:
    key = jax.random.key(seed)
    keys = iter(jax.random.split(key, 32))

    def nrm(shape, scale):
        return jax.random.normal(next(keys), shape, jnp.float32) * scale

    d = D_MODEL
    n_even = (DEPTH + 1) // 2
    n_odd = DEPTH // 2
    return {
        'x': nrm((BATCH, SEQ, d), 1.0),
        'c': nrm((BATCH, d), 1.0),
        'ctx': nrm((BATCH, CTX_LEN, d), 1.0),
        'c_ctx': nrm((d,), 1.0),
        'mod_w': nrm((DEPTH, d, 6 * d), 0.5 * d ** -0.5),
        'mod_b': nrm((DEPTH, 6 * d), 0.02),
        'ln_g': 1.0 + nrm((DEPTH, 2, d), 0.02),
        'ln_b': nrm((DEPTH, 2, d), 0.02),
        'attn_w_in': nrm((n_even, d, ATTN_PROJ_W), d ** -0.5),
        'attn_lambda': nrm((n_even, 4, HEAD_DIM), 0.1),
        'attn_subln_g': 1.0 + nrm((n_even, DIFF_V_DIM), 0.02),
        'attn_sink': nrm((n_even, WIN_Q_HEADS), 1.0),
        'attn_w_out': nrm((n_even, MIX_W, d), MIX_W ** -0.5 * DEEPNORM_BETA),
        'hy_w_in': nrm((n_odd, d, 3 * d), d ** -0.5),
        'hy_conv_w': nrm((n_odd, SHORT_CONV, 3 * d), SHORT_CONV ** -0.5),
        'hy_conv_b': nrm((n_odd, 3 * d), 0.02),
        'hy_ffn_w_in': nrm((n_odd, FILTER_EMB, FILTER_HID), FILTER_EMB ** -0.5),
        'hy_ffn_w_hid': nrm((n_odd, 2, FILTER_HID, FILTER_HID), FILTER_HID ** -0.5),
        'hy_ffn_b': nrm((n_odd, 3, FILTER_HID), 0.02),
        'hy_sin_freq': 1.0 + nrm((n_odd, 3, FILTER_HID), 0.02),
        'hy_ffn_w_out': nrm((n_odd, FILTER_HID, 2 * HYENA_ORDER * d), FILTER_OUT_SCALE * FILTER_HID ** -0.5),
        'hy_skip': nrm((n_odd, HYENA_ORDER, d), 1.0),
        'hy_w_out': nrm((n_odd, d, d), d ** -0.5 * DEEPNORM_BETA),
        'router_w': nrm((d, N_EXPERTS), d ** -0.5),
        'router_bias': nrm((N_EXPERTS,), 0.01),
        'exp_w_gate': nrm((DEPTH, N_EXPERTS, d, EXPERT_FF), d ** -0.5),
        'exp_w_up': nrm((DEPTH, N_EXPERTS, d, EXPERT_FF), d ** -0.5),
        'exp_w_down': nrm((DEPTH, N_EXPERTS, EXPERT_FF, d), EXPERT_FF ** -0.5 * DEEPNORM_BETA),
    }


def reference(x, c, ctx, c_ctx, mod_w, mod_b, ln_g, ln_b, attn_w_in, attn_lambda, attn_subln_g, attn_sink, attn_w_out,
              hy_w_in, hy_conv_w, hy_conv_b, hy_ffn_w_in, hy_ffn_w_hid, hy_ffn_b, hy_sin_freq, hy_ffn_w_out, hy_skip, hy_w_out,
              router_w, router_bias, exp_w_gate, exp_w_up, exp_w_down):
    n = x.shape[1]
    rows = n // GRID_W
    cos, sin = axial_rope_tables(rows)
    xc = ctx
    for i in range(DEPTH):
        is_attn = i % 2 == 0
        ctx_live = any(j % 2 == 0 for j in range(i + 1, DEPTH))
        sh1, sc1, g1, sh2, sc2, g2 = jnp.split((jax.nn.silu(c) @ mod_w[i] + mod_b[i])[:, None, :], 6, axis=-1)
        h = x * (1 + sc1) + sh1
        if is_attn or ctx_live:
            csh1, csc1, cg1, csh2, csc2, cg2 = jnp.split(jax.nn.silu(c_ctx) @ mod_w[i] + mod_b[i], 6, axis=-1)
            hc = xc * (1 + csc1) + csh1
        if is_attn:
            e = i // 2
            lam_init = 0.8 - 0.6 * math.exp(-0.3 * i)
            y, yc = attention_group_mixer(h, hc, attn_w_in[e], attn_lambda[e], attn_subln_g[e], attn_sink[e], attn_w_out[e],
                                          lam_init, cos, sin, ctx_live)
        else:
            o = i // 2
            hy = (hy_w_in[o], hy_conv_w[o], hy_conv_b[o], hy_ffn_w_in[o], hy_ffn_w_hid[o], hy_ffn_b[o], hy_sin_freq[o],
                  hy_ffn_w_out[o], hy_skip[o], hy_w_out[o])
            y = hyena_mixer(h, *hy)
            yc = hyena_mixer(hc, *hy) if ctx_live else None
        x = layer_norm(DEEPNORM_ALPHA * x + g1 * y, ln_g[i, 0], ln_b[i, 0])
        f = moe_ffn(x * (1 + sc2) + sh2, router_w, router_bias, exp_w_gate[i], exp_w_up[i], exp_w_down[i])
        x = layer_norm(DEEPNORM_ALPHA * x + g2 * f, ln_g[i, 1], ln_b[i, 1])
        if ctx_live:
            xc = layer_norm(DEEPNORM_ALPHA * xc + cg1 * yc, ln_g[i, 0], ln_b[i, 0])
            fc = moe_ffn(xc * (1 + csc2) + csh2, router_w, router_bias, exp_w_gate[i], exp_w_up[i], exp_w_down[i])
            xc = layer_norm(DEEPNORM_ALPHA * xc + cg2 * fc, ln_g[i, 1], ln_b[i, 1])
    return x
```

```python
import os
import math
import numpy as np
import ml_dtypes
import concourse.bass as bass
import concourse.mybir as mybir
from concourse.bass_utils import run_bass_kernel_spmd
from contextlib import ExitStack

F32 = mybir.dt.float32
BF16 = mybir.dt.bfloat16
I32 = mybir.dt.int32
ALU = mybir.AluOpType
AF = mybir.ActivationFunctionType

ENGS = ("pe", "act", "dve", "pool", "sp")
NDSEM = 40

D = 1024
L = 2048
C = 256
NT = 16
DEPTH = 2
ALPHA = (2 * DEPTH) ** 0.25
LN_EPS = 1e-5
NE = 16
NFREQ = 2048


class StopBuild(Exception):
    pass


class KB:
    def __init__(self, nc):
        self.nc = nc
        self.es = ExitStack()
        self.q = {e: [] for e in ENGS}
        self.cnt = {e: 0 for e in ENGS}
        self.sem = {e: self.es.enter_context(nc.semaphore("s_" + e)) for e in ENGS}
        self.dsem = [self.es.enter_context(nc.semaphore("d%d" % i)) for i in range(NDSEM)]
        self.dcnt = [0] * NDSEM
        self.dnext = 0
        self.waited = {e: {} for e in ENGS}
        self.lastw = {}
        self.readers = {}
        self.n_ops = 0
        self.psn = 0
        self.pst = None
        self.held = set()

    def sb(self, name, shape, dt=F32, es=None):
        self.uid = getattr(self, "uid", 0) + 1
        return (es or self.es).enter_context(self.nc.sbuf_tensor("%s_u%d" % (name, self.uid), list(shape), dt))

    def init_psum(self):
        self.pst = [self.es.enter_context(self.nc.psum_tensor("ps%d" % i, [128, 512], F32)) for i in range(8)]

    def ps(self):
        while True:
            i = self.psn
            self.psn = (self.psn + 1) % 8
            if i not in self.held:
                return self.pst[i], "ps%d" % i

    def ps_hold(self, n):
        out = []
        for _ in range(n):
            t, k = self.ps()
            self.held.add(int(k[2:]))
            out.append((t, k))
        return out

    def ps_release(self, lst):
        for t, k in lst:
            self.held.discard(int(k[2:]))

    def _semobj(self, sk):
        return self.sem[sk] if isinstance(sk, str) else self.dsem[sk]

    def _deps(self, eng, r, w):
        deps = {}

        def add(d):
            if d is None:
                return
            sk, v = d
            if deps.get(sk, 0) < v:
                deps[sk] = v

        for k in r:
            add(self.lastw.get(k))
        for k in w:
            add(self.lastw.get(k))
            for sk, v in self.readers.get(k, {}).items():
                add((sk, v))
        out = []
        for sk, v in deps.items():
            if sk == eng and eng == "pe":
                continue
            if self.waited[eng].get(sk, 0) >= v:
                continue
            self.waited[eng][sk] = v
            out.append((sk, v))
        return out

    def _commit(self, ev, r, w):
        sk, v = ev
        for k in r:
            d = self.readers.setdefault(k, {})
            if d.get(sk, 0) < v:
                d[sk] = v
        for k in w:
            self.lastw[k] = ev
            self.readers[k] = {}

    def op(self, eng, fn, r=(), w=()):
        w = list(w) + [k for k in r if k.startswith("ps")]
        r = [k for k in r if not k.startswith("ps")]
        waits = self._deps(eng, r, w)
        self.cnt[eng] += 1
        ev = (eng, self.cnt[eng])
        self.q[eng].append((waits, fn, (eng, 1)))
        self._commit(ev, r, w)
        self.n_ops += 1

    def dma(self, eng, fn, r=(), w=()):
        j = self.dnext
        self.dnext = (self.dnext + 1) % NDSEM
        waits = self._deps(eng, r, w)
        prev = self.dcnt[j]
        if prev > 0 and self.waited[eng].get(j, 0) < prev:
            self.waited[eng][j] = prev
            waits.append((j, prev))
        self.dcnt[j] += 16
        ev = (j, self.dcnt[j])
        self.q[eng].append((waits, fn, (j, 16)))
        self._commit(ev, r, w)
        self.n_ops += 1

    def barrier(self):
        for e in ENGS:
            waits = []
            for e2 in ENGS:
                if e2 != e and self.cnt[e2] > self.waited[e].get(e2, 0):
                    self.waited[e][e2] = self.cnt[e2]
                    waits.append((e2, self.cnt[e2]))
            for j in range(NDSEM):
                if self.dcnt[j] > self.waited[e].get(j, 0):
                    self.waited[e][j] = self.dcnt[j]
                    waits.append((j, self.dcnt[j]))
            if waits:
                self.q[e].append((waits, None, None))

    def emit(self):
        nc = self.nc

        def replay(e, name):
            for waits, fn, inc in self.q[name]:
                for sk, v in waits:
                    e.wait_ge(self._semobj(sk), v)
                if fn is not None:
                    ins = fn(e)
                    ins.then_inc(self._semobj(inc[0]), inc[1])

        with nc.Block() as block:
            @block.tensor
            def _(e):
                replay(e, "pe")

            @block.scalar
            def _(e):
                replay(e, "act")

            @block.vector
            def _(e):
                replay(e, "dve")

            @block.gpsimd
            def _(e):
                replay(e, "pool")

            @block.sync
            def _(e):
                replay(e, "sp")
        self.es.close()

    def mm(self, out, lhsT, rhs, start, stop, r, w):
        self.op("pe", lambda e: e.matmul(out, lhsT=lhsT, rhs=rhs, start=start, stop=stop), r=r, w=w)

    def tr(self, out, in_, ident, r, w):
        self.op("pe", lambda e: e.transpose(out, in_, ident), r=list(r) + ["ident"], w=w)

    def act(self, out, in_, func, r, w, bias=None, scale=None, accum_out=None):
        kw = {}
        if bias is not None:
            kw["bias"] = bias
        if scale is not None:
            kw["scale"] = scale
        if accum_out is not None:
            kw["accum_out"] = accum_out
        self.op("act", lambda e: e.activation(out=out, in_=in_, func=func, **kw), r=r, w=w)

    def tt(self, eng, out, in0, in1, op, r, w):
        self.op(eng, lambda e: e.tensor_tensor(out=out, in0=in0, in1=in1, op=op), r=r, w=w)

    def ts(self, eng, out, in0, s1, s2, op0, op1, r, w):
        if op1 is None:
            self.op(eng, lambda e: e.tensor_scalar(out=out, in0=in0, scalar1=s1, scalar2=None, op0=op0), r=r, w=w)
        else:
            self.op(eng, lambda e: e.tensor_scalar(out=out, in0=in0, scalar1=s1, scalar2=s2, op0=op0, op1=op1), r=r, w=w)

    def stt(self, eng, out, in0, scalar, in1, op0, op1, r, w):
        self.op(eng, lambda e: e.scalar_tensor_tensor(out=out, in0=in0, scalar=scalar, in1=in1, op0=op0, op1=op1), r=r, w=w)

    def cp(self, eng, out, in_, r, w):
        if eng == "act":
            self.op("act", lambda e: e.copy(out=out, in_=in_), r=r, w=w)
        else:
            self.op(eng, lambda e: e.tensor_copy(out=out, in_=in_), r=r, w=w)

    def memset(self, eng, ap, val, w):
        self.op(eng, lambda e: e.memset(ap, val), w=w)

    def ld(self, out, in_, w, r=(), eng="sp", slow=False):
        if slow:
            self.dma(eng, lambda e: e.dma_start(out=out, in_=in_, allow_slow_non_contiguous=True), r=r, w=w)
        else:
            self.dma(eng, lambda e: e.dma_start(out=out, in_=in_), r=r, w=w)


def bc_row(t, off, n):
    return bass.AP(t, off, [[0, 128], [1, n]])


def col_view(t, off, nk):
    return bass.AP(t, off, [[1, 128], [128, nk]])


_CONSTS = None


def host_consts():
    global _CONSTS
    if _CONSTS is not None:
        return _CONSTS
    c = {}
    c["ident_f"] = np.eye(128, dtype=np.float32)
    c["ident_b"] = np.eye(128, dtype=np.float32).astype(ml_dtypes.bfloat16)
    R = np.zeros((128, 128), np.float32)
    for m in range(128):
        if m % 64 < 32:
            R[m, m + 32] = -1.0
        else:
            R[m, m - 32] = 1.0
    c["rotT"] = np.ascontiguousarray(R.T).astype(ml_dtypes.bfloat16)
    t = np.arange(L)
    r = (t // 64).astype(np.float32)[:, None]
    col = (t % 64).astype(np.float32)[:, None]
    inv_freq = (10000.0 ** (-np.arange(0, 32, 2, dtype=np.float32) / 32)).astype(np.float32)[None, :]
    ang = np.concatenate([r * inv_freq, col * inv_freq], -1).astype(np.float32)
    ang = np.concatenate([ang, ang], -1)
    cos = np.cos(ang).astype(np.float32)
    sin = np.sin(ang).astype(np.float32)
    c["cosT"] = np.ascontiguousarray(np.concatenate([cos.T, cos.T], 0))
    c["sinT"] = np.ascontiguousarray(np.concatenate([sin.T, sin.T], 0))
    kj = np.arange(128)[:, None]
    qi = np.arange(128)[None, :]
    mL = (kj >= qi).astype(np.float32)
    mU = (kj <= qi).astype(np.float32)
    c["maskL"] = np.tile(mL, (1, 4)).astype(ml_dtypes.bfloat16)
    c["maskU"] = np.tile(mU, (1, 4)).astype(ml_dtypes.bfloat16)
    n = L
    tt_ = np.linspace(0.0, 1.0, n, dtype=np.float32)[:, None]
    bands = 16
    w = (2.0 * math.pi * np.arange(n, dtype=np.float32)[:, None] / n).astype(np.float32)
    fr = np.linspace(1e-4, bands - 1, bands, dtype=np.float32)[None, :]
    z = np.concatenate([tt_, np.cos(fr * w), -np.sin(fr * w)], -1).astype(np.float32)
    c["hy_zT"] = np.ascontiguousarray(z.T)
    deltas = np.abs(np.linspace(math.log(1e-2) / 1.5, math.log(1e-2) / 0.3, D, dtype=np.float32))
    c["hy_decay"] = np.exp(-tt_ * deltas[None, :]).astype(np.float32)
    tau = np.arange(n, dtype=np.float64)[:, None]
    kk = (np.arange(NFREQ, dtype=np.float64) + 0.5)[None, :]
    ph = (2.0 * math.pi / 4096.0) * tau * kk
    Cm = np.cos(ph)
    Sm = np.sin(ph)
    def fwd_l(M):
        return np.ascontiguousarray(M.reshape(16, 128, 16, 128).transpose(2, 1, 0, 3)).astype(ml_dtypes.bfloat16)
    c["dft_c"] = fwd_l(Cm)
    c["dft_ms"] = fwd_l(-Sm)
    Gc = (Cm / 2048.0).T.reshape(16, 128, 16, 128)
    Gs = (-Sm / 2048.0).T.reshape(16, 128, 16, 128)
    G = np.concatenate([Gc, Gs], 0)
    c["idft"] = np.ascontiguousarray(G.transpose(2, 1, 0, 3)).astype(ml_dtypes.bfloat16)
    _CONSTS = c
    return c


def build_program(stage=99, dbg=False, stop=None):
    nc = bass.Bass("TRN2", target_bir_lowering=False)
    T = {}

    def din(name, shape, dt=F32):
        T[name] = nc.dram_tensor(name, list(shape), dt, kind="ExternalInput")
        return T[name]

    def dscr(name, shape, dt=F32):
        T[name] = nc.dram_tensor(name, list(shape), dt, kind="Internal")
        return T[name]

    x_in = din("x", [L, D])
    ctx_in = din("ctx", [C, D])
    cT_in = din("cT", [128, 8])
    cctxT_in = din("cctxT", [128, 8])
    mod_w = din("mod_w", [2, D, 6 * D])
    mod_b = din("mod_b", [2, 6 * D])
    ln_g = din("ln_g", [2, 2, D])
    ln_b = din("ln_b", [2, 2, D])
    attn_w_in = din("attn_w_in", [D, 2304])
    attn_lambda = din("attn_lambda", [1, 256])
    attn_subln_g = din("attn_subln_g", [1, 128])
    attn_sink = din("attn_sink", [1, 8])
    attn_w_out = din("attn_w_out", [D, D])
    ident_f_in = din("ident_f", [128, 128])
    ident_b_in = din("ident_b", [128, 128], BF16)
    rotT_in = din("rotT", [128, 128], BF16)
    cosT_in = din("cosT", [128, L])
    sinT_in = din("sinT", [128, L])
    maskL_in = din("maskL", [128, 512], BF16)
    maskU_in = din("maskU", [128, 512], BF16)
    if stage >= 2:
        router_w = din("router_w", [D, NE])
        router_bias = din("router_bias", [1, NE])
        exp_w_gate = din("exp_w_gate", [2, NE, D, D])
        exp_w_up = din("exp_w_up", [2, NE, D, D])
        exp_w_down = din("exp_w_down", [2, NE, D, D])
    if stage >= 3:
        hy_w_in = din("hy_w_in", [D, 3 * D])
        hy_conv_w = din("hy_conv_w", [3, 3 * D])
        hy_conv_b = din("hy_conv_b", [1, 3 * D])
        hy_ffn_w_in = din("hy_ffn_w_in", [33, 64])
        hy_ffn_w_hid = din("hy_ffn_w_hid", [2, 64, 64])
        hy_ffn_bT = din("hy_ffn_bT", [64, 3])
        hy_sin_freqT = din("hy_sin_freqT", [64, 3])
        hy_ffn_w_out = din("hy_ffn_w_out", [64, 4 * D])
        hy_skip = din("hy_skip", [2, D])
        hy_w_out = din("hy_w_out", [D, D])
        hy_zT = din("hy_zT", [33, L])
        hy_decay = din("hy_decay", [L, D])
        dft_c = din("dft_c", [16, 128, 16, 128], BF16)
        dft_ms = din("dft_ms", [16, 128, 16, 128], BF16)
        idft = din("idft", [16, 128, 32, 128], BF16)
        u_scr = dscr("u_scr", [L, 3 * D])
        vb_scr = dscr("vb_scr", [L, D], BF16)
        sd_scr = dscr("sd_scr", [2, L, 2 * D], BF16)
        Hspec = dscr("Hspec", [2, 16, 128, 2 * D])
        z2_scr = dscr("z2_scr", [L, D])
        z2b_scr = dscr("z2b_scr", [L, D], BF16)
    out_t = nc.dram_tensor("out", [L, D], F32, kind="ExternalOutput")
    modrow = dscr("modrow", [2, 6 * D])
    cmodrow = dscr("cmodrow", [1, 2 * D])

    kb = KB(nc)
    kb.init_psum()
    P = kb.es

    ident_f = kb.sb("ident_f_s", [128, 128])
    ident_b = kb.sb("ident_b_s", [128, 128], BF16)
    kb.ld(ident_f[:], ident_f_in.ap(), w=["ident"])
    kb.ld(ident_b[:], ident_b_in.ap(), w=["identb"])
    x = kb.sb("xres", [128, NT, D])
    g_bc = kb.sb("g_bc", [128, D])
    lng_bc = kb.sb("lng_bc", [128, D])
    lnb_bc = kb.sb("lnb_bc", [128, D])
    cols = kb.sb("modcols", [128, 6, 8])
    ccols = kb.sb("cmodcols", [128, 2, 8])
    small = kb.sb("small", [128, 64])
    eps_t = kb.sb("eps_t", [128, 1])
    kb.memset("dve", eps_t[:], LN_EPS, w=["eps"])

    def mod_phase(i, with_ctx):
        with ExitStack() as es:
            sc = kb.sb("silu_c", [128, 8], es=es)
            scc = kb.sb("silu_cc", [128, 8], es=es)
            mb_row = kb.sb("mb_row", [1, 6 * D], es=es)
            orow = kb.sb("orow", [1, 6 * D], es=es)
            corow = kb.sb("corow", [1, 2 * D], es=es)
            wbuf = [kb.sb("modw%d" % j, [128, 8, 512], es=es) for j in range(2)]
            kb.ld(sc[:], cT_in.ap(), w=["silu_c"])
            kb.act(sc[:], sc[:], AF.Silu, r=["silu_c"], w=["silu_c"])
            kb.ld(mb_row[:], mod_b.ap()[i:i + 1, :], w=["mb_row"])
            if with_ctx:
                kb.ld(scc[:], cctxT_in.ap(), w=["silu_cc"])
                kb.act(scc[:], scc[:], AF.Silu, r=["silu_cc"], w=["silu_cc"])
            wv = mod_w.ap()[i].rearrange("(k p) n -> p k n", p=128)
            for n in range(12):
                wb = wbuf[n % 2]
                wk = "modw%d" % (n % 2)
                kb.ld(wb[:], wv[:, :, n * 512:(n + 1) * 512], w=[wk])
                pt, pk = kb.ps()
                for k in range(8):
                    kb.mm(pt[0:1, :], sc[:, k:k + 1], wb[:, k, :], k == 0, k == 7, r=["silu_c", wk], w=[pk])
                kb.tt("dve", orow[0:1, n * 512:(n + 1) * 512], pt[0:1, :], mb_row[0:1, n * 512:(n + 1) * 512], ALU.add,
                      r=[pk, "mb_row"], w=["orow"])
                if with_ctx and n < 4:
                    pt2, pk2 = kb.ps()
                    for k in range(8):
                        kb.mm(pt2[0:1, :], scc[:, k:k + 1], wb[:, k, :], k == 0, k == 7, r=["silu_cc", wk], w=[pk2])
                    kb.tt("dve", corow[0:1, n * 512:(n + 1) * 512], pt2[0:1, :], mb_row[0:1, n * 512:(n + 1) * 512], ALU.add,
                          r=[pk2, "mb_row"], w=["corow"])
            kb.ld(modrow.ap()[i:i + 1, :], orow[:], r=["orow"], w=["modrow%d" % i])
            if with_ctx:
                kb.ld(cmodrow.ap(), corow[:], r=["corow"], w=["cmodrow"])
            for j in (0, 1, 3, 4):
                kb.ld(cols[:, j, :], col_view(modrow, i * 6 * D + j * D, 8), r=["modrow%d" % i], w=["cols%d" % j], slow=True)
            for j in (1, 4):
                kb.ts("dve", cols[:, j, :], cols[:, j, :], 1.0, None, ALU.add, None, r=["cols%d" % j], w=["cols%d" % j])
            if with_ctx:
                for j in (0, 1):
                    kb.ld(ccols[:, j, :], col_view(cmodrow, j * D, 8), r=["cmodrow"], w=["ccols%d" % j], slow=True)
                kb.ts("dve", ccols[:, 1, :], ccols[:, 1, :], 1.0, None, ALU.add, None, r=["ccols1"], w=["ccols1"])
        kb.barrier()

    def load_gate_ln(i, which):
        goff = i * 6 * D + (2 if which == 0 else 5) * D
        kb.ld(g_bc[:], bc_row(modrow, goff, D), r=["modrow%d" % i], w=["g_bc"])
        kb.ld(lng_bc[:], bc_row(ln_g, (i * 2 + which) * D, D), w=["lng_bc"])
        kb.ld(lnb_bc[:], bc_row(ln_b, (i * 2 + which) * D, D), w=["lnb_bc"])

    def ln_inplace(t, es_tmp):
        junk, st = es_tmp
        xt = x[:, t, :]
        xk = "x%d" % t
        kb.memset("dve", st[:, 0:2], 0.0, w=["lnst"])
        kb.act(junk[:], xt, AF.Identity, r=[xk, "lnst"], w=["tmp_o0", "tmp_o1", "lnst_a"], accum_out=st[:, 0:1])
        kb.act(junk[:], xt, AF.Square, r=[xk, "lnst"], w=["tmp_o0", "tmp_o1", "lnst_b"], accum_out=st[:, 1:2])
        kb.ts("dve", st[:, 2:4], st[:, 0:2], 1.0 / D, None, ALU.mult, None, r=["lnst_a", "lnst_b", "lnst"], w=["lnst2"])
        kb.tt("dve", st[:, 4:5], st[:, 2:3], st[:, 2:3], ALU.mult, r=["lnst2"], w=["lnst3"])
        kb.tt("dve", st[:, 5:6], st[:, 3:4], st[:, 4:5], ALU.subtract, r=["lnst2", "lnst3"], w=["lnst4"])
        kb.act(st[:, 6:7], st[:, 5:6], AF.Sqrt, r=["lnst4", "eps"], w=["lnst5"], bias=eps_t[:, 0:1])
        kb.op("dve", lambda e: e.reciprocal(out=st[:, 7:8], in_=st[:, 6:7]), r=["lnst5"], w=["lnst6"])
        kb.ts("dve", xt, xt, st[:, 2:3], st[:, 7:8], ALU.subtract, ALU.mult, r=[xk, "lnst2", "lnst6"], w=[xk])
        kb.tt("dve", xt, xt, lng_bc[:], ALU.mult, r=[xk, "lng_bc"], w=[xk])
        kb.tt("dve", xt, xt, lnb_bc[:], ALU.add, r=[xk, "lnb_bc"], w=[xk])

    def attention_layer():
        i = 0
        lam_init = 0.8 - 0.6 * math.exp(-0.3 * i)
        xflat = x.bitcast(BF16)[:].rearrange("p a b -> p (a b)")
        hT = xflat[:, 0:8 * (L + C)].rearrange("p (k n) -> p k n", k=8)
        xin_f = x[:, 10:14, :]
        with ExitStack() as es:
            qaT = kb.sb("qaT", [128, 4, L], BF16, es=es)
            kaT = kb.sb("kaT", [128, 4, L + C], BF16, es=es)
            qwT = kb.sb("qwT", [128, 4, L], BF16, es=es)
            kwT = kb.sb("kwT", [128, 2, L + C], BF16, es=es)
            va = kb.sb("va", [128, 18, 4, 130], BF16, es=es)
            vw = kb.sb("vw", [128, 18, 2, 66], BF16, es=es)
            rotT = kb.sb("rotT_s", [128, 128], BF16, es=es)
            kb.ld(rotT[:], rotT_in.ap(), w=["rotT"])
            kb.memset("pool", va[:].rearrange("p a b c -> p (a b c)"), 1.0, w=["va%d" % t for t in range(18)])
            kb.memset("pool", vw[:].rearrange("p a b c -> p (a b c)"), 1.0, w=["vw%d" % t for t in range(18)])
            with ExitStack() as es2:
                cs = [kb.sb("cs%d" % j, [128, 2, 512], es=es2) for j in range(2)]
                for g in range(5):
                    xb = xin_f
                    xk = "xin"
                    nt = 4 if g < 4 else 2
                    if g < 4:
                        kb.ld(xb[:, :, :], x_in.ap()[g * 512:(g + 1) * 512, :].rearrange("(t p) d -> p t d", p=128), w=[xk])
                    else:
                        kb.ld(xb[:, 0:2, :], ctx_in.ap().rearrange("(t p) d -> p t d", p=128), w=[xk])
                    ccol = cols if g < 4 else ccols
                    ck = ["cols0", "cols1"] if g < 4 else ["ccols0", "ccols1"]
                    for k in range(8):
                        pt, pk = kb.ps()
                        for j in range(nt):
                            kb.tr(pt[:, j * 128:(j + 1) * 128], xb[:, j, k * 128:(k + 1) * 128], ident_f[:], r=[xk], w=[pk])
                        kb.act(hT[:, k, g * 512:g * 512 + nt * 128], pt[:, 0:nt * 128], AF.Identity, r=[pk] + ck, w=["hT%d_%d" % (k, g)],
                               bias=ccol[:, 0, k:k + 1], scale=ccol[:, 1, k:k + 1])
                if stop == "A":
                    return
                wst = [kb.sb("wst%d" % j, [128, 8, 128], BF16, es=es2) for j in range(3)]
                wstf = [kb.sb("wstf%d" % j, [128, 8, 128], F32, es=es2) for j in range(2)]
                qb = [kb.sb("qb%d" % j, [128, 512], BF16, es=es2) for j in range(2)]
                t1 = [kb.sb("rt1_%d" % j, [128, 512], es=es2) for j in range(2)]
                t2 = [kb.sb("rt2_%d" % j, [128, 512], es=es2) for j in range(2)]
                w_in_v = attn_w_in.ap().rearrange("(k p) n -> p k n", p=128)
                jobs = []
                for h in range(4):
                    jobs.append(("qa", h, [(h * 128, 128)], qaT, False))
                for h in range(4):
                    jobs.append(("qw", h, [(512 + h * 128, 128)], qwT, False))
                for h in range(4):
                    jobs.append(("ka", h, [(1024 + h * 128, 128)], kaT, True))
                for g in range(2):
                    jobs.append(("kw", g, [(2048 + g * 64, 64), (2048 + g * 64, 64)], kwT, True))
                cnt = 0
                for cch in range(5):
                    n0 = cch * 512
                    nn = 512 if cch < 4 else 256
                    if cch < 4:
                        csb = cs[cch % 2]
                        csk = "cs%d" % (cch % 2)
                        kb.ld(csb[:, 0, :], cosT_in.ap()[:, n0:n0 + 512], w=[csk])
                        kb.ld(csb[:, 1, :], sinT_in.ap()[:, n0:n0 + 512], w=[csk])
                    for ji, (nm, idx, colspec, dest, has_ctx) in enumerate(jobs):
                        if cch == 4 and not has_ctx:
                            continue
                        ws = wst[cnt % 3]
                        wk = "wst%d" % (cnt % 3)
                        o = 0
                        wf = wstf[cnt % 2]
                        wfk = "wstf%d" % (cnt % 2)
                        for (c0, cn) in colspec:
                            kb.ld(wf[:, :, o:o + cn], w_in_v[:, :, c0:c0 + cn], w=[wfk])
                            o += cn
                        kb.cp("pool", ws[:], wf[:], r=[wfk], w=[wk])
                        pa, pak = kb.ps()
                        for k in range(8):
                            kb.mm(pa[:, 0:nn], ws[:, k, :], hT[:, k, n0:n0 + nn], k == 0, k == 7, r=[wk, "hT%d_%d" % (k, cch)], w=[pak])
                        dk = "%s%d_%d" % (nm, idx, cch)
                        b = cnt % 2
                        cnt += 1
                        if stop == "B1":
                            kb.cp("act", qb[b][:, 0:nn], pa[:, 0:nn], r=[pak], w=["qb%d" % b])
                            continue
                        if stop in ("B2", "B2a", "B2b"):
                            if cch == 4:
                                continue
                            kb.cp("act", qb[b][:], pa[:], r=[pak], w=["qb%d" % b])
                            if stop != "B2b":
                                pb, pbk = kb.ps()
                                kb.mm(pb[:], rotT[:], qb[b][:], True, True, r=["rotT", "qb%d" % b], w=[pbk])
                            if stop != "B2a":
                                kb.tt("dve", t1[b][:], pa[:], csb[:, 0, :], ALU.mult, r=[pak, csk, "qb%d" % b], w=["rt1_%d" % b])
                            if stop == "B2":
                                kb.tt("dve", t2[b][:], pb[:], csb[:, 1, :], ALU.mult, r=[pbk, csk], w=["rt2_%d" % b])
                            continue
                        if cch == 4:
                            kb.cp("act", dest[:, idx, n0:n0 + nn], pa[:, 0:nn], r=[pak], w=[dk])
                            continue
                        kb.cp("act", qb[b][:], pa[:], r=[pak], w=["qb%d" % b])
                        pb, pbk = kb.ps()
                        kb.mm(pb[:], rotT[:], qb[b][:], True, True, r=["rotT", "qb%d" % b], w=[pbk])
                        kb.tt("dve", t1[b][:], pa[:], csb[:, 0, :], ALU.mult, r=[pak, csk], w=["rt1_%d" % b])
                        kb.tt("dve", t2[b][:], pb[:], csb[:, 1, :], ALU.mult, r=[pbk, csk], w=["rt2_%d" % b])
                        kb.tt("dve", dest[:, idx, n0:n0 + 512], t1[b][:], t2[b][:], ALU.add, r=["rt1_%d" % b, "rt2_%d" % b], w=[dk])
                if stop in ("B", "B1", "B2", "B2a", "B2b"):
                    return
                wv = kb.sb("wv", [128, 8, 640], BF16, es=es2)
                for (d0, c0, cn) in ((0, 1536, 128), (128, 1664, 128), (256, 1792, 128), (384, 1920, 128), (512, 2176, 128)):
                    wf = wstf[cnt % 2]
                    wfk = "wstf%d" % (cnt % 2)
                    cnt += 1
                    kb.ld(wf[:], w_in_v[:, :, c0:c0 + cn], w=[wfk])
                    kb.cp("pool", wv[:, :, d0:d0 + cn], wf[:], r=[wfk], w=["wv"])
                for t in range(18):
                    g = t // 4
                    p1, p1k = kb.ps()
                    p2, p2k = kb.ps()
                    for k in range(8):
                        kb.mm(p1[:], hT[:, k, t * 128:(t + 1) * 128], wv[:, k, 0:512], k == 0, k == 7, r=["wv", "hT%d_%d" % (k, g)], w=[p1k])
                    for k in range(8):
                        kb.mm(p2[:, 0:128], hT[:, k, t * 128:(t + 1) * 128], wv[:, k, 512:640], k == 0, k == 7, r=["wv", "hT%d_%d" % (k, g)], w=[p2k])
                    kb.cp("act" if t % 2 == 0 else "dve", va[:, t, :, 0:128], p1[:].rearrange("p (h d) -> p h d", h=4), r=[p1k], w=["va%d" % t])
                    kb.cp("dve" if t % 2 == 0 else "act", vw[:, t, :, 0:64], p2[:, 0:128].rearrange("p (h d) -> p h d", h=2), r=[p2k], w=["vw%d" % t])
            kb.barrier()
            if stop == "V":
                return
            with ExitStack() as es3:
                catT = kb.sb("catT", [128, 8, 512], BF16, es=es3)
                tmpw = x[:, 14:16, :].rearrange("p a (k n) -> p (a k) n", k=4)
                w_out_b = kb.sb("w_out_b", [128, 8, D], BF16, es=es3)
                w_out_v = attn_w_out.ap().rearrange("(k p) n -> p k n", p=128)
                for cc in range(4):
                    kb.ld(tmpw, w_out_v[:, :, cc * 256:(cc + 1) * 256], w=["x14", "x15"])
                    kb.cp("pool", w_out_b[:, :, cc * 256:(cc + 1) * 256], tmpw, r=["x14", "x15"], w=["w_out_b"])
                if stop == "C0a":
                    return
                lamt = kb.sb("lamt", [128, 256], es=es3)
                lam2 = kb.sb("lam2", [128, 16], es=es3)
                kb.ld(lamt[:], bc_row(attn_lambda, 0, 256), w=["lamt"])
                kb.tt("dve", lamt[:, 0:64], lamt[:, 0:64], lamt[:, 64:128], ALU.mult, r=["lamt"], w=["lamt"])
                kb.tt("dve", lamt[:, 128:192], lamt[:, 128:192], lamt[:, 192:256], ALU.mult, r=["lamt"], w=["lamt"])
                kb.memset("dve", lam2[:], 0.0, w=["lam2"])
                kb.act(lamt[:, 64:128], lamt[:, 0:64], AF.Identity, r=["lamt", "lam2"], w=["lamt", "lam2a"], accum_out=lam2[:, 0:1])
                kb.act(lamt[:, 192:256], lamt[:, 128:192], AF.Identity, r=["lamt", "lam2"], w=["lamt", "lam2b"], accum_out=lam2[:, 1:2])
                kb.act(lam2[:, 2:4], lam2[:, 0:2], AF.Exp, r=["lam2a", "lam2b"], w=["lam2c"])
                kb.tt("dve", lam2[:, 4:5], lam2[:, 3:4], lam2[:, 2:3], ALU.subtract, r=["lam2c"], w=["lam2d"])
                kb.ts("dve", lam2[:, 5:6], lam2[:, 4:5], -lam_init, None, ALU.add, None, r=["lam2d"], w=["neglam"])
                if stop == "C0b":
                    return
                sg = kb.sb("sg", [128, 1], es=es3)
                kb.ld(sg[:], bass.AP(attn_subln_g, 0, [[1, 128], [1, 1]]), w=["sg"], slow=True)
                kb.ts("dve", sg[:], sg[:], 1.0 - lam_init, None, ALU.mult, None, r=["sg"], w=["sg"])
                sk = kb.sb("sinkexp", [128, 8], es=es3)
                kb.ld(sk[:], bc_row(attn_sink, 0, 8), w=["sink"])
                kb.act(sk[:], sk[:], AF.Exp, r=["sink"], w=["sink"])
                if stop == "C0c":
                    return
                maskL = kb.sb("maskL_s", [128, 512], BF16, es=es3)
                maskU = kb.sb("maskU_s", [128, 512], BF16, es=es3)
                kb.ld(maskL[:], maskL_in.ap(), w=["maskL"])
                kb.ld(maskU[:], maskU_in.ap(), w=["maskU"])
                ptb = [kb.sb("ptb%d" % j, [128, 512], BF16, es=es3) for j in range(4)]
                o0n = kb.sb("o0n", [128, 4, 128], es=es3)
                oa = kb.sb("oa", [128, 4, 128], es=es3)
                junk = kb.sb("junk_a", [128, 128], es=es3)
                st = kb.sb("st_a", [128, 4, 8], es=es3)
                owb = kb.sb("owb", [128, 512], es=es3)
                tmp = kb.sb("tmp_o", [128, D], es=es3)
                st2 = kb.sb("st_ln", [128, 8], es=es3)
                load_gate_ln(0, 0)
                pcnt = 0
                if stop == "C0":
                    return
                for qc in range(4):
                    for h in range(4):
                        if stop == "C1" and h == 1:
                            return
                        if stop in ("W1", "W1a", "W1b", "W1c", "W2", "W3"):
                            break
                        for m in range(2):
                            accs = kb.ps_hold(4)
                            for kt in range(18):
                                ps_s, psk = kb.ps()
                                kch = kt // 4
                                kb.mm(ps_s[:], kaT[m * 64:(m + 1) * 64, h, kt * 128:(kt + 1) * 128], qaT[m * 64:(m + 1) * 64, h, qc * 512:(qc + 1) * 512],
                                      True, True, r=["ka%d_%d" % (h, kch), "qa%d_%d" % (h, qc)], w=[psk])
                                pb_ = ptb[pcnt % 4]
                                pbk = "ptb%d" % (pcnt % 4)
                                pcnt += 1
                                kb.act(pb_[:], ps_s[:], AF.Exp, r=[psk], w=[pbk], scale=0.125)
                                for s in range(4):
                                    kb.mm(accs[s][0][:, 0:129], pb_[:, s * 128:(s + 1) * 128], va[:, kt, h, 0:129], kt == 0, kt == 17,
                                          r=[pbk, "va%d" % kt], w=[accs[s][1]])
                            for s in range(4):
                                ac, ack = accs[s]
                                sk_ = "st_a%d" % s
                                kb.op("dve", lambda e, ac=ac, s=s: e.reciprocal(out=st[:, s, 0:1], in_=ac[:, 128:129]), r=[ack], w=[sk_])
                                if m == 0:
                                    kb.ts("dve", o0n[:, s, :], ac[:, 0:128], st[:, s, 0:1], None, ALU.mult, None, r=[ack, sk_], w=["o0n%d" % s])
                                else:
                                    kb.tt("dve", st[:, s, 1:2], st[:, s, 0:1], lam2[:, 5:6], ALU.mult, r=[sk_, "neglam"], w=[sk_])
                                    kb.stt("dve", oa[:, s, :], ac[:, 0:128], st[:, s, 1:2], o0n[:, s, :], ALU.mult, ALU.add,
                                           r=[ack, sk_, "o0n%d" % s], w=["oa%d" % s])
                                    kb.memset("dve", st[:, s, 2:3], 0.0, w=[sk_])
                                    kb.act(junk[:], oa[:, s, :], AF.Square, r=["oa%d" % s, sk_], w=["junk_a", sk_], accum_out=st[:, s, 2:3])
                                    kb.act(st[:, s, 3:4], st[:, s, 2:3], AF.Sqrt, r=[sk_, "eps"], w=[sk_], bias=eps_t[:, 0:1], scale=1.0 / 128)
                                    kb.op("dve", lambda e, s=s: e.reciprocal(out=st[:, s, 4:5], in_=st[:, s, 3:4]), r=[sk_], w=[sk_])
                                    kb.ts("dve", oa[:, s, :], oa[:, s, :], st[:, s, 4:5], None, ALU.mult, None, r=["oa%d" % s, sk_], w=["oa%d" % s])
                            kb.ps_release(accs)
                            if m == 1:
                                ptr, ptrk = kb.ps()
                                for s in range(4):
                                    kb.tr(ptr[:, s * 128:(s + 1) * 128], oa[:, s, :], ident_f[:], r=["oa%d" % s], w=[ptrk])
                                kb.act(catT[:, h, :], ptr[:], AF.Identity, r=[ptrk, "sg"], w=["cat%d" % h], scale=sg[:, 0:1])
                    for nl in range(4):
                        n = 4 * qc + nl
                        if stop in ("C2", "W3") and nl == 2:
                            return
                        for g in range(2):
                            kts = []
                            if n >= 1:
                                kts.append((n - 1, "L"))
                            kts.append((n, None))
                            if n <= 14:
                                kts.append((n + 1, "U"))
                            kts.append((16, None))
                            kts.append((17, None))
                            accs = kb.ps_hold(4)
                            for ki, (kt, mt) in enumerate(kts):
                                kch = kt // 4
                                pb_ = ptb[pcnt % 4]
                                pbk = "ptb%d" % (pcnt % 4)
                                pcnt += 1
                                for p in range(2):
                                    ps_s, psk = kb.ps()
                                    for jj in range(2):
                                        j = 2 * jj + p
                                        head = 4 * g + j
                                        ti = head // 2
                                        kb.mm(ps_s[:, jj * 128:(jj + 1) * 128], kwT[p * 64:(p + 1) * 64, g, kt * 128:(kt + 1) * 128],
                                              qwT[p * 64:(p + 1) * 64, ti, n * 128:(n + 1) * 128], True, True,
                                              r=["kw%d_%d" % (g, kch), "qw%d_%d" % (ti, qc)], w=[psk])
                                    kb.act(pb_[:, p * 256:(p + 1) * 256], ps_s[:, 0:256], AF.Exp, r=[psk], w=[pbk], scale=0.125)
                                if mt is not None and stop != "W1a":
                                    mk = maskL if mt == "L" else maskU
                                    kb.tt("dve", pb_[:], pb_[:], mk[:], ALU.mult, r=[pbk, "maskL", "maskU"], w=[pbk])
                                if stop == "W1b":
                                    continue
                                for j in range(4):
                                    sl = (j % 2) * 2 + j // 2
                                    kb.mm(accs[j][0][:, 0:65], pb_[:, sl * 128:(sl + 1) * 128], vw[:, kt, g, 0:65], ki == 0, ki == len(kts) - 1,
                                          r=[pbk, "vw%d" % kt], w=[accs[j][1]])
                            for j in range(4):
                                if stop in ("W1b", "W1c"):
                                    continue
                                head = 4 * g + j
                                ac, ack = accs[j]
                                sk_ = "st_a%d" % j
                                kb.tt("dve", st[:, j, 0:1], ac[:, 64:65], sk[:, head:head + 1], ALU.add, r=[ack, "sink"], w=[sk_])
                                kb.op("dve", lambda e, j=j: e.reciprocal(out=st[:, j, 1:2], in_=st[:, j, 0:1]), r=[sk_], w=[sk_])
                                kb.ts("dve", owb[:, head * 64:(head + 1) * 64], ac[:, 0:64], st[:, j, 1:2], None, ALU.mult, None, r=[ack, sk_], w=["owb%d" % head])
                            kb.ps_release(accs)
                            if stop in ("W1", "W1a", "W1b", "W1c"):
                                return
                        if stop == "W2":
                            return
                        ptr, ptrk = kb.ps()
                        for tI in range(4):
                            kb.tr(ptr[:, tI * 128:(tI + 1) * 128], owb[:, tI * 128:(tI + 1) * 128], ident_f[:], r=["owb%d" % (2 * tI), "owb%d" % (2 * tI + 1)], w=[ptrk])
                        kb.cp("act", catT[:, 4:8, nl * 128:(nl + 1) * 128], ptr[:].rearrange("p (t q) -> p t q", t=4), r=[ptrk], w=["catw%d" % nl])
                    if stop == "C3" and qc == 1:
                        return
                    for nl in range(4):
                        t = 4 * qc + nl
                        xk = "x%d" % t
                        kb.ld(x[:, t, :], x_in.ap()[t * 128:(t + 1) * 128, :], w=[xk])
                        rk = ["cat%d" % h for h in range(4)] + ["catw%d" % nl, "w_out_b"]
                        for cchunk in range(2):
                            py, pyk = kb.ps()
                            for k in range(8):
                                kb.mm(py[:], catT[:, k, nl * 128:(nl + 1) * 128], w_out_b[:, k, cchunk * 512:(cchunk + 1) * 512], k == 0, k == 7, r=rk, w=[pyk])
                            kb.tt("dve", tmp[:, cchunk * 512:(cchunk + 1) * 512], py[:], g_bc[:, cchunk * 512:(cchunk + 1) * 512], ALU.mult,
                                  r=[pyk, "g_bc"], w=["tmp_o%d" % cchunk])
                        kb.stt("dve", x[:, t, :], x[:, t, :], ALPHA, tmp[:], ALU.mult, ALU.add, r=[xk, "tmp_o0", "tmp_o1"], w=[xk, "tmp_o0", "tmp_o1"])
                        ln_inplace(t, (tmp, st2))
            kb.barrier()

    def bl(ap2, n):
        a = ap2.ap
        return bass.AP(ap2.tensor, ap2.offset, [list(a[0]), list(a[1]), [0, n]])

    def bm(ap2, n):
        a = ap2.ap
        return bass.AP(ap2.tensor, ap2.offset, [list(a[0]), [0, n], list(a[1])])

    def moe_layer(i):
        load_gate_ln(i, 1)
        BIG = 1.0e9
        with ExitStack() as es:
            xmT = kb.sb("xmT", [128, 8, L], BF16, es=es)
            wts = kb.sb("wts", [128, NT, NE], es=es)
            with ExitStack() as esA:
                xmf = kb.sb("xmf", [128, 8, 512], es=esA)
                rw = kb.sb("rw", [128, 8, NE], es=esA)
                rb = kb.sb("rb_bc", [128, NE], es=esA)
                lg = kb.sb("lg", [128, NT, NE], es=esA)
                sc_ = kb.sb("rsc", [128, NT, NE], es=esA)
                sel = kb.sb("rsel", [128, NT, NE], es=esA)
                m1t = kb.sb("rm1", [128, NT, NE], es=esA)
                m2t = kb.sb("rm2", [128, NT, NE], es=esA)
                r16 = kb.sb("r16", [128, 8, NT], es=esA)
                g64 = kb.sb("g64", [128, 10, 64], es=esA)
                kb.ld(rw[:], router_w.ap().rearrange("(k p) e -> p k e", p=128), w=["rw"])
                kb.ld(rb[:], bc_row(router_bias, 0, NE), w=["rb"])
                for g in range(4):
                    for k in range(8):
                        pt, pk = kb.ps()
                        for j in range(4):
                            kb.tr(pt[:, j * 128:(j + 1) * 128], x[:, 4 * g + j, k * 128:(k + 1) * 128], ident_f[:], r=["x%d" % (4 * g + j)], w=[pk])
                        kb.act(xmT[:, k, g * 512:(g + 1) * 512], pt[:], AF.Identity, r=[pk, "cols3", "cols4"], w=["xmT%d_%d" % (k, g)],
                               bias=cols[:, 3, k:k + 1], scale=cols[:, 4, k:k + 1])
                        kb.ts("dve", xmf[:, k, :], pt[:], cols[:, 4, k:k + 1], cols[:, 3, k:k + 1], ALU.mult, ALU.add, r=[pk, "cols3", "cols4"], w=["xmf%d" % k])
                    for j in range(4):
                        pl, plk = kb.ps()
                        for k in range(8):
                            kb.mm(pl[:, 0:NE], xmf[:, k, j * 128:(j + 1) * 128], rw[:, k, :], k == 0, k == 7, r=["xmf%d" % k, "rw"], w=[plk])
                        kb.cp("dve", lg[:, 4 * g + j, :], pl[:, 0:NE], r=[plk], w=["lg"])
                R = lambda *k: list(k)
                mx = r16[:, 0, :]
                kb.op("dve", lambda e: e.tensor_reduce(out=mx, in_=lg[:], axis=mybir.AxisListType.X, op=ALU.max), r=["lg"], w=["r16_0"])
                kb.tt("dve", sc_[:], lg[:], bl(mx, NE), ALU.subtract, r=["lg", "r16_0"], w=["rsc"])
                kb.act(sc_[:], sc_[:], AF.Exp, r=["rsc"], w=["rsc"])
                ssum = r16[:, 1, :]
                kb.op("dve", lambda e: e.tensor_reduce(out=ssum, in_=sc_[:], axis=mybir.AxisListType.X, op=ALU.add), r=["rsc"], w=["r16_1"])
                kb.op("dve", lambda e: e.reciprocal(out=r16[:, 2, :], in_=ssum), r=["r16_1"], w=["r16_2"])
                kb.tt("dve", sc_[:], sc_[:], bl(r16[:, 2, :], NE), ALU.mult, r=["rsc", "r16_2"], w=["rsc"])
                kb.tt("dve", sel[:], sc_[:], bm(rb[:], NT), ALU.add, r=["rsc", "rb"], w=["rsel"])
                selflat = sel[:].rearrange("p t e -> p (t e)")

                def gview(j):
                    a = selflat.ap
                    return bass.AP(selflat.tensor, selflat.offset + j, [list(a[0]), [4, 64]])
                hi1, lo1, hi2, lo2, top1, mn, mxl, sec, gs, gm = [g64[:, q, :] for q in range(10)]
                kb.tt("dve", hi1, gview(0), gview(1), ALU.max, r=["rsel"], w=["g0"])
                kb.tt("dve", lo1, gview(0), gview(1), ALU.min, r=["rsel"], w=["g1"])
                kb.tt("dve", hi2, gview(2), gview(3), ALU.max, r=["rsel"], w=["g2"])
                kb.tt("dve", lo2, gview(2), gview(3), ALU.min, r=["rsel"], w=["g3"])
                kb.tt("dve", top1, hi1, hi2, ALU.max, r=["g0", "g2"], w=["g4"])
                kb.tt("dve", mn, hi1, hi2, ALU.min, r=["g0", "g2"], w=["g5"])
                kb.tt("dve", mxl, lo1, lo2, ALU.max, r=["g1", "g3"], w=["g6"])
                kb.tt("dve", sec, mn, mxl, ALU.max, r=["g5", "g6"], w=["g7"])
                kb.tt("dve", gs, top1, sec, ALU.add, r=["g4", "g7"], w=["g8"])
                gmax = r16[:, 3, :]
                kb.op("dve", lambda e: e.tensor_reduce(out=gmax, in_=gs.rearrange("p (t g) -> p t g", g=4), axis=mybir.AxisListType.X, op=ALU.max), r=["g8"], w=["r16_3"])
                kb.tt("dve", gm.rearrange("p (t g) -> p t g", g=4), gs.rearrange("p (t g) -> p t g", g=4), bl(gmax, 4), ALU.is_equal, r=["g8", "r16_3"], w=["g9"])
                kb.ts("dve", gm, gm, -1.0, BIG, ALU.add, ALU.mult, r=["g9"], w=["g9"])
                kb.tt("dve", m1t[:].rearrange("p t (g q) -> p (t g) q", q=4), sel[:].rearrange("p t (g q) -> p (t g) q", q=4), bl(gm, 4), ALU.add,
                      r=["rsel", "g9"], w=["rm1"])
                mx1 = r16[:, 4, :]
                kb.op("dve", lambda e: e.tensor_reduce(out=mx1, in_=m1t[:], axis=mybir.AxisListType.X, op=ALU.max), r=["rm1"], w=["r16_4"])
                kb.tt("dve", m2t[:], m1t[:], bl(mx1, NE), ALU.is_equal, r=["rm1", "r16_4"], w=["rm2"])
                kb.stt("dve", m1t[:], m2t[:], -BIG, m1t[:], ALU.mult, ALU.add, r=["rm1", "rm2"], w=["rm1"])
                mx2 = r16[:, 5, :]
                kb.op("dve", lambda e: e.tensor_reduce(out=mx2, in_=m1t[:], axis=mybir.AxisListType.X, op=ALU.max), r=["rm1"], w=["r16_5"])
                kb.tt("dve", m1t[:], m1t[:], bl(mx2, NE), ALU.is_equal, r=["rm1", "r16_5"], w=["rm1"])
                kb.tt("dve", m2t[:], m2t[:], m1t[:], ALU.add, r=["rm1", "rm2"], w=["rm2"])
                kb.tt("dve", m2t[:], m2t[:], sc_[:], ALU.mult, r=["rm2", "rsc"], w=["rm2"])
                ws_ = r16[:, 6, :]
                kb.op("dve", lambda e: e.tensor_reduce(out=ws_, in_=m2t[:], axis=mybir.AxisListType.X, op=ALU.add), r=["rm2"], w=["r16_6"])
                kb.op("dve", lambda e: e.reciprocal(out=r16[:, 7, :], in_=ws_), r=["r16_6"], w=["r16_7"])
                kb.tt("dve", wts[:], m2t[:], bl(r16[:, 7, :], NE), ALU.mult, r=["rm2", "r16_7"], w=["wts"])
                for t in range(NT):
                    kb.ts("dve", x[:, t, :], x[:, t, :], ALPHA, None, ALU.mult, None, r=["x%d" % t], w=["x%d" % t])
            kb.barrier()
            if stop == "route":
                kb.cp("dve", x[:, 0, 0:256], wts[:].rearrange("p t e -> p (t e)"), r=["wts"], w=["x0"])
                return
            with ExitStack() as esB:
                NP = 8
                wp = [kb.sb("ewp%d" % j, [128, 8, 128], BF16, es=esB) for j in range(NP)]
                Wdb = [kb.sb("ewd%d" % j, [128, 8, D], BF16, es=esB) for j in range(2)]
                stg = [kb.sb("estg%d" % j, [128, 8, 128], es=esB) for j in range(2)]
                HTt = kb.sb("HTt", [128, 8, L], BF16, es=esB)
                sgt = [kb.sb("sgt%d" % j, [128, 512], es=esB) for j in range(2)]
                pcn = 0
                scnt = 0
                ceng = ("pool", "act", "pool", "dve")

                def stage_piece(src_view, pc, dst_ap, dst_key, scale_g):
                    nonlocal scnt
                    sg_ = stg[scnt % 2]
                    sgk = "estg%d" % (scnt % 2)
                    eng = ceng[scnt % 4]
                    scnt += 1
                    kb.ld(sg_[:], src_view[:, :, pc * 128:(pc + 1) * 128], w=[sgk])
                    if scale_g:
                        kb.tt("pool" if eng == "act" else eng, dst_ap, sg_[:], bm(g_bc[:, pc * 128:(pc + 1) * 128], 8), ALU.mult, r=[sgk, "g_bc"], w=[dst_key])
                    else:
                        kb.cp(eng, dst_ap, sg_[:], r=[sgk], w=[dst_key])

                for e_ in range(NE):
                    gv = exp_w_gate.ap()[i, e_].rearrange("(k p) n -> p k n", p=128)
                    uv = exp_w_up.ap()[i, e_].rearrange("(k p) n -> p k n", p=128)
                    dv = exp_w_down.ap()[i, e_].rearrange("(k p) n -> p k n", p=128)
                    Wd = Wdb[e_ % 2]
                    Wdk = "ewd%d" % (e_ % 2)
                    for pc in range(8):
                        stage_piece(dv, pc, Wd[:, :, pc * 128:(pc + 1) * 128], Wdk, True)
                    for f in range(8):
                        wg_ = wp[pcn % NP]
                        wgk = "ewp%d" % (pcn % NP)
                        pcn += 1
                        wu_ = wp[pcn % NP]
                        wuk = "ewp%d" % (pcn % NP)
                        pcn += 1
                        stage_piece(gv, f, wg_[:], wgk, False)
                        stage_piece(uv, f, wu_[:], wuk, False)
                        for tc in range(4):
                            pg, pgk = kb.ps()
                            pu, puk = kb.ps()
                            for k in range(8):
                                kb.mm(pg[:], wg_[:, k, :], xmT[:, k, tc * 512:(tc + 1) * 512], k == 0, k == 7, r=[wgk, "xmT%d_%d" % (k, tc)], w=[pgk])
                            for k in range(8):
                                kb.mm(pu[:], wu_[:, k, :], xmT[:, k, tc * 512:(tc + 1) * 512], k == 0, k == 7, r=[wuk, "xmT%d_%d" % (k, tc)], w=[puk])
                            st_ = sgt[tc % 2]
                            stk = "sgt%d" % (tc % 2)
                            kb.act(st_[:], pg[:], AF.Silu, r=[pgk], w=[stk])
                            kb.tt("dve", HTt[:, f, tc * 512:(tc + 1) * 512], st_[:], pu[:], ALU.mult, r=[stk, puk], w=["HT%d_%d" % (f, tc)])
                    for tc in range(4):
                        for j in range(4):
                            t = 4 * tc + j
                            for c in range(2):
                                py, pyk = kb.ps()
                                for f in range(8):
                                    kb.mm(py[:], HTt[:, f, t * 128:(t + 1) * 128], Wd[:, f, c * 512:(c + 1) * 512], f == 0, f == 7, r=["HT%d_%d" % (f, tc), Wdk], w=[pyk])
                                kb.stt("dve", x[:, t, c * 512:(c + 1) * 512], py[:], wts[:, t, e_:e_ + 1], x[:, t, c * 512:(c + 1) * 512], ALU.mult, ALU.add,
                                       r=[pyk, "wts", "x%d" % t], w=["x%d" % t])
            kb.barrier()
            with ExitStack() as esC:
                junkL = kb.sb("junkL", [128, D], es=esC)
                stL = kb.sb("stL", [128, 8], es=esC)
                for t in range(NT):
                    ln_inplace(t, (junkL, stL))
        kb.barrier()

    def hyena_layer():
        i = 1
        PI = math.pi
        with ExitStack() as es:
            hT = kb.sb("hTp", [128, 8, L + 2], BF16, es=es)
            kb.memset("pool", hT[:, :, 0:1], 0.0, w=["hTp_l"])
            kb.memset("pool", hT[:, :, L + 1:L + 2], 0.0, w=["hTp_r"])
            for g in range(4):
                for k in range(8):
                    pt, pk = kb.ps()
                    for j in range(4):
                        kb.tr(pt[:, j * 128:(j + 1) * 128], x[:, 4 * g + j, k * 128:(k + 1) * 128], ident_f[:], r=["x%d" % (4 * g + j)], w=[pk])
                    kb.act(hT[:, k, 1 + g * 512:1 + (g + 1) * 512], pt[:], AF.Identity, r=[pk, "cols0", "cols1"], w=["hTp%d_%d" % (k, g)],
                           bias=cols[:, 0, k:k + 1], scale=cols[:, 1, k:k + 1])
            hkeys = ["hTp_l", "hTp_r"] + ["hTp%d_%d" % (k, g) for k in range(8) for g in range(4)]
            Wj2 = [[kb.sb("hyW%d_%d" % (j, q), [128, 8, 512], BF16, es=es) for j in range(3)] for q in range(2)]
            cw2 = [kb.sb("hycw%d" % q, [128, 4, 512], es=es) for q in range(2)]
            stg = [kb.sb("hystg%d" % j, [128, 8, 128], es=es) for j in range(2)]
            ev = [kb.sb("hyev%d" % j, [128, 512], es=es) for j in range(2)]
            evb = [kb.sb("hyevb%d" % j, [128, 512], BF16, es=es) for j in range(2)]
            wv = hy_w_in.ap().rearrange("(k p) n -> p k n", p=128)
            scnt = 0
            ecnt = 0
            for c in range(6):
                Wj = Wj2[c % 2]
                wq = c % 2
                cw = cw2[wq]
                cwk = "hycw%d" % wq
                for j in range(3):
                    kb.ld(cw[:, j, :], bc_row(hy_conv_w, j * 3 * D + c * 512, 512), w=[cwk])
                kb.ld(cw[:, 3, :], bc_row(hy_conv_b, c * 512, 512), w=[cwk])
                for pc in range(4):
                    sg_ = stg[scnt % 2]
                    sgk = "hystg%d" % (scnt % 2)
                    scnt += 1
                    kb.ld(sg_[:], wv[:, :, c * 512 + pc * 128:c * 512 + (pc + 1) * 128], w=[sgk])
                    for j in range(3):
                        kb.tt(("pool", "dve", "pool")[j], Wj[j][:, :, pc * 128:(pc + 1) * 128], sg_[:], bm(cw[:, j, pc * 128:(pc + 1) * 128], 8), ALU.mult,
                              r=[sgk, cwk], w=["hyW%d_%d" % (j, wq)])
                for t in range(NT):
                    pu, puk = kb.ps()
                    n = 0
                    for j in range(3):
                        for k in range(8):
                            kb.mm(pu[:], hT[:, k, t * 128 + j:t * 128 + j + 128], Wj[j][:, k, :], n == 0, n == 23, r=hkeys + ["hyW%d_%d" % (j, wq)], w=[puk])
                            n += 1
                    eb = ev[ecnt % 2]
                    ebk = "hyev%d" % (ecnt % 2)
                    kb.tt("dve", eb[:], pu[:], cw[:, 3, :], ALU.add, r=[puk, cwk], w=[ebk])
                    kb.ld(u_scr.ap()[t * 128:(t + 1) * 128, c * 512:(c + 1) * 512], eb[:], r=[ebk], w=["u_scr"])
                    if c < 2:
                        ebb = evb[ecnt % 2]
                        ebbk = "hyevb%d" % (ecnt % 2)
                        kb.cp("pool", ebb[:], eb[:], r=[ebk], w=[ebbk])
                        kb.ld(vb_scr.ap()[t * 128:(t + 1) * 128, c * 512:(c + 1) * 512], ebb[:], r=[ebbk], w=["vb_scr"])
                    ecnt += 1
        kb.barrier()
        if stop == "hy2":
            return
        with ExitStack() as es:
            zT = kb.sb("hyzT", [33, L], es=es)
            w1 = kb.sb("hyw1", [33, 64], es=es)
            wh = kb.sb("hywh", [64, 2, 64], es=es)
            wo = kb.sb("hywo", [64, 4 * D], es=es)
            bT = kb.sb("hybT", [64, 3], es=es)
            sfT = kb.sb("hysfT", [64, 3], es=es)
            hid = [kb.sb("hyhid%d" % j, [64, L], es=es) for j in range(2)]
            msk = kb.sb("hymsk", [64, L], es=es)
            negpi = kb.sb("hynegpi", [64, 1], es=es)
            kb.ld(zT[:], hy_zT.ap(), w=["hyzT"])
            kb.ld(w1[:], hy_ffn_w_in.ap(), w=["hyw1"])
            kb.ld(wh[:], hy_ffn_w_hid.ap().rearrange("l i o -> i l o"), w=["hywh"])
            kb.ld(wo[:], hy_ffn_w_out.ap(), w=["hywo"])
            kb.ld(bT[:], hy_ffn_bT.ap(), w=["hybT"])
            kb.ld(sfT[:], hy_sin_freqT.ap(), w=["hysfT"])
            cur = None
            for l in range(3):
                dst = hid[l % 2]
                dk = "hyhid%d" % (l % 2)
                for q in range(4):
                    pp, ppk = kb.ps()
                    if l == 0:
                        kb.mm(pp[0:64, :], w1[:, :], zT[:, q * 512:(q + 1) * 512], True, True, r=["hyw1", "hyzT"], w=[ppk])
                    else:
                        kb.mm(pp[0:64, :], wh[:, l - 1, :], cur[:, q * 512:(q + 1) * 512], True, True, r=["hywh", curk], w=[ppk])
                    kb.ts("dve", dst[:, q * 512:(q + 1) * 512], pp[0:64, :], bT[:, l:l + 1], sfT[:, l:l + 1], ALU.add, ALU.mult, r=[ppk, "hybT", "hysfT"], w=[dk])
                for rep in range(2):
                    kb.ts("dve", msk[:], dst[:], -PI, 2 * PI, ALU.is_lt, ALU.mult, r=[dk], w=["hymsk"])
                    kb.ts("pool", dst[:], dst[:], PI, None, ALU.is_gt, None, r=[dk, "hymsk"], w=[dk + "g"]) if False else None
                    kb.stt("dve", msk[:], dst[:], PI, msk[:], ALU.is_gt, ALU.subtract, r=[dk, "hymsk"], w=["hymsk"]) if False else None
                    kb.tt("dve", dst[:], dst[:], msk[:], ALU.add, r=[dk, "hymsk"], w=[dk])
                    kb.ts("dve", msk[:], dst[:], PI, -2 * PI, ALU.is_gt, ALU.mult, r=[dk], w=["hymsk"])
                    kb.tt("dve", dst[:], dst[:], msk[:], ALU.add, r=[dk, "hymsk"], w=[dk])
                kb.act(dst[:], dst[:], AF.Sin, r=[dk], w=[dk])
                cur = dst
                curk = dk
            fd = kb.sb("hyfd", [128, 2 * D], es=es)
            bd = kb.sb("hybd", [128, 2 * D], es=es)
            sb_ = [kb.sb("hysb%d" % j, [128, 2 * D], BF16, es=es) for j in range(2)]
            dec = kb.sb("hydec", [128, D], es=es)
            for a in range(16):
                kb.ld(dec[:], hy_decay.ap()[a * 128:(a + 1) * 128, :], w=["hydec"])
                for q in range(8):
                    pf, pfk = kb.ps()
                    kb.mm(pf[:], cur[:, a * 128:(a + 1) * 128], wo[:, q * 512:(q + 1) * 512], True, True, r=[curk, "hywo"], w=[pfk])
                    half = q % 2
                    tgt = fd if q < 4 else bd
                    tk = "hyfd" if q < 4 else "hybd"
                    kb.tt("dve", tgt[:, (q % 4) * 512:(q % 4 + 1) * 512], pf[:], dec[:, half * 512:(half + 1) * 512], ALU.mult, r=[pfk, "hydec"], w=[tk])
                if a == 0:
                    kb.memset("dve", bd[0:1, :], 0.0, w=["hybd"])
                kb.tt("dve", sb_[0][:], fd[:], bd[:], ALU.add, r=["hyfd", "hybd"], w=["hysb0"])
                kb.tt("pool", sb_[1][:], fd[:], bd[:], ALU.subtract, r=["hyfd", "hybd"], w=["hysb1"])
                kb.ld(sd_scr.ap()[0, a * 128:(a + 1) * 128, :], sb_[0][:], r=["hysb0"], w=["sd_scr"])
                kb.ld(sd_scr.ap()[1, a * 128:(a + 1) * 128, :], sb_[1][:], r=["hysb1"], w=["sd_scr"])
        kb.barrier()
        with ExitStack() as es:
            sdt = kb.sb("hysdt", [128, 16, 512], BF16, es=es)
            ft = [kb.sb("hyft%d" % j, [128, 16, 128], BF16, es=es) for j in range(2)]
            he = [kb.sb("hyhe%d" % j, [128, 512], es=es) for j in range(2)]
            fcnt = 0
            for ri, dmat in enumerate((dft_c, dft_ms)):
                for cc in range(4):
                    kb.ld(sdt[:], sd_scr.ap()[ri].rearrange("(a p) n -> p a n", p=128)[:, :, cc * 512:(cc + 1) * 512], r=["sd_scr"], w=["hysdt"])
                    for kt in range(16):
                        f_ = ft[fcnt % 2]
                        fk = "hyft%d" % (fcnt % 2)
                        if fcnt == 0:
                            kb.ld(f_[:], dmat.ap()[kt], w=[fk])
                        nxt = fcnt + 1
                        if nxt < 128:
                            n_ri, n_kt = nxt // 64, nxt % 16
                            kb.ld(ft[nxt % 2][:], (dft_c, dft_ms)[n_ri].ap()[n_kt], w=["hyft%d" % (nxt % 2)])
                        ph, phk = kb.ps()
                        for a in range(16):
                            kb.mm(ph[:], f_[:, a, :], sdt[:, a, :], a == 0, a == 15, r=[fk, "hysdt"], w=[phk])
                        h_ = he[fcnt % 2]
                        hk = "hyhe%d" % (fcnt % 2)
                        fcnt += 1
                        kb.cp("act", h_[:], ph[:], r=[phk], w=[hk])
                        kb.ld(Hspec.ap()[ri, kt, :, cc * 512:(cc + 1) * 512], h_[:], r=[hk], w=["Hspec"])
        kb.barrier()
        with ExitStack() as es:
            z3T = kb.sb("hyz3T", [128, 8, L], BF16, es=es)
            zin = kb.sb("hyzin", [128, 16, 512], BF16, es=es)
            Pm = kb.sb("hyP", [128, 32, 512], BF16, es=es)
            ftcs = [kb.sb("hyftc%d" % j, [128, 16, 128], BF16, es=es) for j in range(2)]
            ftss = [kb.sb("hyfts%d" % j, [128, 16, 128], BF16, es=es) for j in range(2)]
            gts = [kb.sb("hygt%d" % j, [128, 32, 128], BF16, es=es) for j in range(2)]
            Ht = kb.sb("hyHt", [128, 2, 512], es=es)
            tmp = [kb.sb("hytmp%d" % j, [128, 512], es=es) for j in range(2)]
            tmp = [tmp[0], tmp[1], tmp[0], tmp[1]]
            dbc = 0
            gate = kb.sb("hygate", [128, 512], es=es)
            zp = kb.sb("hyzp", [128, 512], es=es)
            zo = kb.sb("hyzo", [128, 512], es=es)
            zob = kb.sb("hyzob", [128, 512], BF16, es=es)
            skb = kb.sb("hyskb", [128, 512], es=es)
            for o in range(2):
                src_b = vb_scr if o == 0 else z2b_scr
                src_f = u_scr if o == 0 else z2_scr
                for hf in range(2):
                    c0 = hf * 512
                    kb.ld(zin[:], src_b.ap().rearrange("(a p) n -> p a n", p=128)[:, :, c0:c0 + 512], r=["vb_scr", "z2b_scr"], w=["hyzin"])
                    kb.ld(skb[:], bc_row(hy_skip, o * D + c0, 512), w=["hyskb"])
                    kb.ld(ftcs[0][:], dft_c.ap()[0], w=["hyftc0"])
                    kb.ld(ftss[0][:], dft_ms.ap()[0], w=["hyfts0"])
                    for kt in range(16):
                        ftc = ftcs[kt % 2]
                        fts = ftss[kt % 2]
                        fck = "hyftc%d" % (kt % 2)
                        fsk = "hyfts%d" % (kt % 2)
                        if kt + 1 < 16:
                            kb.ld(ftcs[(kt + 1) % 2][:], dft_c.ap()[kt + 1], w=["hyftc%d" % ((kt + 1) % 2)])
                            kb.ld(ftss[(kt + 1) % 2][:], dft_ms.ap()[kt + 1], w=["hyfts%d" % ((kt + 1) % 2)])
                        kb.ld(Ht[:, 0, :], Hspec.ap()[0, kt, :, o * D + c0:o * D + c0 + 512], r=["Hspec"], w=["hyHt"])
                        kb.ld(Ht[:, 1, :], Hspec.ap()[1, kt, :, o * D + c0:o * D + c0 + 512], r=["Hspec"], w=["hyHt"])
                        pr, prk = kb.ps()
                        pi_, pik = kb.ps()
                        for a in range(16):
                            kb.mm(pr[:], ftc[:, a, :], zin[:, a, :], a == 0, a == 15, r=[fck, "hyzin"], w=[prk])
                        for a in range(16):
                            kb.mm(pi_[:], fts[:, a, :], zin[:, a, :], a == 0, a == 15, r=[fsk, "hyzin"], w=[pik])
                        kb.tt("dve", tmp[0][:], pr[:], Ht[:, 0, :], ALU.mult, r=[prk, "hyHt"], w=["hytmp0"])
                        kb.tt("dve", tmp[1][:], pi_[:], Ht[:, 1, :], ALU.mult, r=[pik, "hyHt"], w=["hytmp1"])
                        kb.tt("dve", Pm[:, kt, :], tmp[0][:], tmp[1][:], ALU.subtract, r=["hytmp0", "hytmp1"], w=["hyP%d" % kt])
                        kb.tt("dve", tmp[2][:], pr[:], Ht[:, 1, :], ALU.mult, r=[prk, "hyHt"], w=["hytmp0"])
                        kb.tt("dve", tmp[3][:], pi_[:], Ht[:, 0, :], ALU.mult, r=[pik, "hyHt"], w=["hytmp1"])
                        kb.tt("dve", Pm[:, 16 + kt, :], tmp[2][:], tmp[3][:], ALU.add, r=["hytmp0", "hytmp1"], w=["hyP%d" % (16 + kt)])
                    pkeys = ["hyP%d" % q for q in range(32)]
                    kb.ld(gts[0][:], idft.ap()[0], w=["hygt0"])
                    for t in range(NT):
                        gt = gts[t % 2]
                        gtk = "hygt%d" % (t % 2)
                        if t + 1 < NT:
                            kb.ld(gts[(t + 1) % 2][:], idft.ap()[t + 1], w=["hygt%d" % ((t + 1) % 2)])
                        py, pyk = kb.ps()
                        for f in range(32):
                            kb.mm(py[:], gt[:, f, :], Pm[:, f, :], f == 0, f == 31, r=[gtk] + pkeys, w=[pyk])
                        gcol = (1 + o) * D + c0
                        kb.ld(gate[:], u_scr.ap()[t * 128:(t + 1) * 128, gcol:gcol + 512], r=["u_scr"], w=["hygate"])
                        kb.ld(zp[:], src_f.ap()[t * 128:(t + 1) * 128, c0:c0 + 512], r=["u_scr", "z2_scr"], w=["hyzp"])
                        kb.tt("dve", zp[:], zp[:], skb[:], ALU.mult, r=["hyzp", "hyskb"], w=["hyzp"])
                        kb.tt("dve", zo[:], py[:], zp[:], ALU.add, r=[pyk, "hyzp"], w=["hyzo"])
                        kb.tt("dve", zo[:], zo[:], gate[:], ALU.mult, r=["hyzo", "hygate"], w=["hyzo"])
                        if o == 0:
                            kb.ld(z2_scr.ap()[t * 128:(t + 1) * 128, c0:c0 + 512], zo[:], r=["hyzo"], w=["z2_scr"])
                            kb.cp("act", zob[:], zo[:], r=["hyzo"], w=["hyzob"])
                            kb.ld(z2b_scr.ap()[t * 128:(t + 1) * 128, c0:c0 + 512], zob[:], r=["hyzob"], w=["z2b_scr"])
                        else:
                            pz, pzk = kb.ps()
                            for j in range(4):
                                kb.tr(pz[:, j * 128:(j + 1) * 128], zo[:, j * 128:(j + 1) * 128], ident_f[:], r=["hyzo"], w=[pzk])
                            kb.cp("act", z3T[:, hf * 4:(hf + 1) * 4, t * 128:(t + 1) * 128], pz[:].rearrange("p (j q) -> p j q", j=4), r=[pzk], w=["hyz3T%d" % t])
            kb.barrier()
            load_gate_ln(1, 0)
            wob = Pm[:, 0:16, :].rearrange("p a n -> p (a n)").rearrange("p (k n) -> p k n", k=8)
            wov = hy_w_out.ap().rearrange("(k p) n -> p k n", p=128)
            stgo = zin[:].bitcast(F32) if False else None
            for cc in range(8):
                kb.ld(Ht[:].rearrange("p a n -> p (a n)").rearrange("p (k n) -> p k n", k=8), wov[:, :, cc * 128:(cc + 1) * 128], w=["hyHt"])
                kb.cp("pool", wob[:, :, cc * 128:(cc + 1) * 128], Ht[:].rearrange("p a n -> p (a n)").rearrange("p (k n) -> p k n", k=8), r=["hyHt"], w=["hywob"])
            tmpo = zin.bitcast(F32)[:].rearrange("p a b -> p (a b)")[:, 0:D]
            stL = kb.sb("hystL", [128, 8], es=es)
            for t in range(NT):
                for c in range(2):
                    py, pyk = kb.ps()
                    for k in range(8):
                        kb.mm(py[:], z3T[:, k, t * 128:(t + 1) * 128], wob[:, k, c * 512:(c + 1) * 512], k == 0, k == 7, r=["hyz3T%d" % t, "hywob"], w=[pyk])
                    kb.tt("dve", tmpo[:, c * 512:(c + 1) * 512], py[:], g_bc[:, c * 512:(c + 1) * 512], ALU.mult, r=[pyk, "g_bc"], w=["tmp_o%d" % c])
                kb.stt("dve", x[:, t, :], x[:, t, :], ALPHA, tmpo[:], ALU.mult, ALU.add, r=["x%d" % t, "tmp_o0", "tmp_o1"], w=["x%d" % t, "tmp_o0", "tmp_o1"])
                ln_inplace(t, (tmpo, stL))
        kb.barrier()

    mod_phase(0, True)
    if stop == "mod":
        kb.ld(out_t.ap()[0:128, 0:48], cols[:].rearrange("p a b -> p (a b)"), r=["cols%d" % j for j in (0, 1, 3, 4)], w=["o"])
        kb.ld(out_t.ap()[128:256, 0:16], ccols[:].rearrange("p a b -> p (a b)"), r=["ccols0", "ccols1"], w=["o2"])
        kb.barrier()
        kb.emit()
        return nc, list(T.keys())
    attention_layer()
    kb.barrier()
    if stage >= 2:
        moe_layer(0)
    if stage >= 3:
        mod_phase(1, False)
        hyena_layer()
    if stage >= 4:
        moe_layer(1)

    for t in range(NT):
        kb.ld(out_t.ap()[t * 128:(t + 1) * 128, :], x[:, t, :], r=["x%d" % t], w=["out%d" % t])
    kb.barrier()
    print("ops", kb.n_ops, {e: len(kb.q[e]) for e in ENGS})
    kb.emit()
    return nc, list(T.keys())


def make_inputs(inputs, names, b):
    c = host_consts()
    m = {}
    for n in names:
        if n in ("modrow", "cmodrow", "u_scr", "vb_scr", "sd_scr", "Hspec", "z2_scr", "z2b_scr"):
            continue
        if n in c:
            m[n] = c[n]
        elif n == "x":
            m[n] = np.ascontiguousarray(inputs["x"][b])
        elif n == "ctx":
            m[n] = np.ascontiguousarray(inputs["ctx"][b])
        elif n == "cT":
            m[n] = np.ascontiguousarray(inputs["c"][b].reshape(8, 128).T)
        elif n == "cctxT":
            m[n] = np.ascontiguousarray(inputs["c_ctx"].reshape(8, 128).T)
        elif n == "attn_w_in":
            m[n] = inputs["attn_w_in"][0]
        elif n == "attn_lambda":
            m[n] = inputs["attn_lambda"][0].reshape(1, 256)
        elif n == "attn_subln_g":
            m[n] = inputs["attn_subln_g"][0].reshape(1, 128)
        elif n == "attn_sink":
            m[n] = inputs["attn_sink"][0].reshape(1, 8)
        elif n == "attn_w_out":
            m[n] = inputs["attn_w_out"][0]
        elif n == "router_bias":
            m[n] = inputs["router_bias"].reshape(1, NE)
        elif n in ("hy_w_in", "hy_conv_w", "hy_ffn_w_in", "hy_ffn_w_hid", "hy_ffn_w_out", "hy_skip", "hy_w_out"):
            m[n] = inputs[n][0]
        elif n == "hy_conv_b":
            m[n] = inputs[n][0].reshape(1, 3 * D)
        elif n == "hy_ffn_bT":
            m[n] = np.ascontiguousarray(inputs["hy_ffn_b"][0].T)
        elif n == "hy_sin_freqT":
            m[n] = np.ascontiguousarray(inputs["hy_sin_freq"][0].T)
        else:
            m[n] = inputs[n]
        m[n] = np.ascontiguousarray(m[n])
    return m


def kernel(**inputs):
    inputs = {k: np.asarray(v) for k, v in inputs.items()}
    nc, names = build_program()
    in_maps = [make_inputs(inputs, names, b) for b in range(8)]
    res = run_bass_kernel_spmd(nc, in_maps, core_ids=list(range(8)))
    out = np.stack([r["out"] for r in res.results], 0)
    return out.astype(np.float32)
```

```python
import os
import math
import numpy as np
import ml_dtypes
import concourse.bass as bass
import concourse.mybir as mybir
from concourse.bass_utils import run_bass_kernel_spmd
from contextlib import ExitStack

F32 = mybir.dt.float32
BF16 = mybir.dt.bfloat16
I32 = mybir.dt.int32
ALU = mybir.AluOpType
AF = mybir.ActivationFunctionType

ENGS = ("pe", "act", "dve", "pool", "sp")
NDSEM = 40

D = 1024
L = 2048
C = 256
NT = 16
DEPTH = 2
ALPHA = (2 * DEPTH) ** 0.25
LN_EPS = 1e-5
NE = 16
NFREQ = 2048


class StopBuild(Exception):
    pass


class KB:
    def __init__(self, nc):
        self.nc = nc
        self.es = ExitStack()
        self.q = {e: [] for e in ENGS}
        self.cnt = {e: 0 for e in ENGS}
        self.sem = {e: self.es.enter_context(nc.semaphore("s_" + e)) for e in ENGS}
        self.dsem = [self.es.enter_context(nc.semaphore("d%d" % i)) for i in range(NDSEM)]
        self.dcnt = [0] * NDSEM
        self.dnext = 0
        self.waited = {e: {} for e in ENGS}
        self.lastw = {}
        self.readers = {}
        self.n_ops = 0
        self.psn = 0
        self.pst = None
        self.held = set()

    def sb(self, name, shape, dt=F32, es=None):
        self.uid = getattr(self, "uid", 0) + 1
        return (es or self.es).enter_context(self.nc.sbuf_tensor("%s_u%d" % (name, self.uid), list(shape), dt))

    def init_psum(self):
        self.pst = [self.es.enter_context(self.nc.psum_tensor("ps%d" % i, [128, 512], F32)) for i in range(8)]

    def ps(self):
        while True:
            i = self.psn
            self.psn = (self.psn + 1) % 8
            if i not in self.held:
                return self.pst[i], "ps%d" % i

    def ps_hold(self, n):
        out = []
        for _ in range(n):
            t, k = self.ps()
            self.held.add(int(k[2:]))
            out.append((t, k))
        return out

    def ps_release(self, lst):
        for t, k in lst:
            self.held.discard(int(k[2:]))

    def _semobj(self, sk):
        return self.sem[sk] if isinstance(sk, str) else self.dsem[sk]

    def _deps(self, eng, r, w):
        deps = {}

        def add(d):
            if d is None:
                return
            sk, v = d
            if deps.get(sk, 0) < v:
                deps[sk] = v

        for k in r:
            add(self.lastw.get(k))
        for k in w:
            add(self.lastw.get(k))
            for sk, v in self.readers.get(k, {}).items():
                add((sk, v))
        out = []
        for sk, v in deps.items():
            if sk == eng and eng == "pe":
                continue
            if self.waited[eng].get(sk, 0) >= v:
                continue
            self.waited[eng][sk] = v
            out.append((sk, v))
        return out

    def _commit(self, ev, r, w):
        sk, v = ev
        for k in r:
            d = self.readers.setdefault(k, {})
            if d.get(sk, 0) < v:
                d[sk] = v
        for k in w:
            self.lastw[k] = ev
            self.readers[k] = {}

    def op(self, eng, fn, r=(), w=()):
        w = list(w) + [k for k in r if k.startswith("ps")]
        r = [k for k in r if not k.startswith("ps")]
        waits = self._deps(eng, r, w)
        self.cnt[eng] += 1
        ev = (eng, self.cnt[eng])
        self.q[eng].append((waits, fn, (eng, 1)))
        self._commit(ev, r, w)
        self.n_ops += 1

    def dma(self, eng, fn, r=(), w=()):
        j = self.dnext
        self.dnext = (self.dnext + 1) % NDSEM
        waits = self._deps(eng, r, w)
        prev = self.dcnt[j]
        if prev > 0 and self.waited[eng].get(j, 0) < prev:
            self.waited[eng][j] = prev
            waits.append((j, prev))
        self.dcnt[j] += 16
        ev = (j, self.dcnt[j])
        self.q[eng].append((waits, fn, (j, 16)))
        self._commit(ev, r, w)
        self.n_ops += 1

    def barrier(self):
        for e in ENGS:
            waits = []
            for e2 in ENGS:
                if e2 != e and self.cnt[e2] > self.waited[e].get(e2, 0):
                    self.waited[e][e2] = self.cnt[e2]
                    waits.append((e2, self.cnt[e2]))
            for j in range(NDSEM):
                if self.dcnt[j] > self.waited[e].get(j, 0):
                    self.waited[e][j] = self.dcnt[j]
                    waits.append((j, self.dcnt[j]))
            if waits:
                self.q[e].append((waits, None, None))

    def emit(self):
        nc = self.nc

        def replay(e, name):
            for waits, fn, inc in self.q[name]:
                for sk, v in waits:
                    e.wait_ge(self._semobj(sk), v)
                if fn is not None:
                    ins = fn(e)
                    ins.then_inc(self._semobj(inc[0]), inc[1])

        with nc.Block() as block:
            @block.tensor
            def _(e):
                replay(e, "pe")

            @block.scalar
            def _(e):
                replay(e, "act")

            @block.vector
            def _(e):
                replay(e, "dve")

            @block.gpsimd
            def _(e):
                replay(e, "pool")

            @block.sync
            def _(e):
                replay(e, "sp")
        self.es.close()

    def mm(self, out, lhsT, rhs, start, stop, r, w):
        self.op("pe", lambda e: e.matmul(out, lhsT=lhsT, rhs=rhs, start=start, stop=stop), r=r, w=w)

    def tr(self, out, in_, ident, r, w):
        self.op("pe", lambda e: e.transpose(out, in_, ident), r=list(r) + ["ident"], w=w)

    def act(self, out, in_, func, r, w, bias=None, scale=None, accum_out=None):
        kw = {}
        if bias is not None:
            kw["bias"] = bias
        if scale is not None:
            kw["scale"] = scale
        if accum_out is not None:
            kw["accum_out"] = accum_out
        self.op("act", lambda e: e.activation(out=out, in_=in_, func=func, **kw), r=r, w=w)

    def tt(self, eng, out, in0, in1, op, r, w):
        self.op(eng, lambda e: e.tensor_tensor(out=out, in0=in0, in1=in1, op=op), r=r, w=w)

    def ts(self, eng, out, in0, s1, s2, op0, op1, r, w):
        if op1 is None:
            self.op(eng, lambda e: e.tensor_scalar(out=out, in0=in0, scalar1=s1, scalar2=None, op0=op0), r=r, w=w)
        else:
            self.op(eng, lambda e: e.tensor_scalar(out=out, in0=in0, scalar1=s1, scalar2=s2, op0=op0, op1=op1), r=r, w=w)

    def stt(self, eng, out, in0, scalar, in1, op0, op1, r, w):
        self.op(eng, lambda e: e.scalar_tensor_tensor(out=out, in0=in0, scalar=scalar, in1=in1, op0=op0, op1=op1), r=r, w=w)

    def cp(self, eng, out, in_, r, w):
        if eng == "act":
            self.op("act", lambda e: e.copy(out=out, in_=in_), r=r, w=w)
        else:
            self.op(eng, lambda e: e.tensor_copy(out=out, in_=in_), r=r, w=w)

    def memset(self, eng, ap, val, w):
        self.op(eng, lambda e: e.memset(ap, val), w=w)

    def ld(self, out, in_, w, r=(), eng="sp", slow=False):
        if slow:
            self.dma(eng, lambda e: e.dma_start(out=out, in_=in_, allow_slow_non_contiguous=True), r=r, w=w)
        else:
            self.dma(eng, lambda e: e.dma_start(out=out, in_=in_), r=r, w=w)


def bc_row(t, off, n):
    return bass.AP(t, off, [[0, 128], [1, n]])


def col_view(t, off, nk):
    return bass.AP(t, off, [[1, 128], [128, nk]])


_CONSTS = None


def host_consts():
    global _CONSTS
    if _CONSTS is not None:
        return _CONSTS
    c = {}
    c["ident_f"] = np.eye(128, dtype=np.float32)
    c["ident_b"] = np.eye(128, dtype=np.float32).astype(ml_dtypes.bfloat16)
    R = np.zeros((128, 128), np.float32)
    for m in range(128):
        if m % 64 < 32:
            R[m, m + 32] = -1.0
        else:
            R[m, m - 32] = 1.0
    c["rotT"] = np.ascontiguousarray(R.T).astype(ml_dtypes.bfloat16)
    t = np.arange(L)
    r = (t // 64).astype(np.float32)[:, None]
    col = (t % 64).astype(np.float32)[:, None]
    inv_freq = (10000.0 ** (-np.arange(0, 32, 2, dtype=np.float32) / 32)).astype(np.float32)[None, :]
    ang = np.concatenate([r * inv_freq, col * inv_freq], -1).astype(np.float32)
    ang = np.concatenate([ang, ang], -1)
    cos = np.cos(ang).astype(np.float32)
    sin = np.sin(ang).astype(np.float32)
    c["cosT"] = np.ascontiguousarray(np.concatenate([cos.T, cos.T], 0))
    c["sinT"] = np.ascontiguousarray(np.concatenate([sin.T, sin.T], 0))
    kj = np.arange(128)[:, None]
    qi = np.arange(128)[None, :]
    mL = (kj >= qi).astype(np.float32)
    mU = (kj <= qi).astype(np.float32)
    c["maskL"] = np.tile(mL, (1, 4)).astype(ml_dtypes.bfloat16)
    c["maskU"] = np.tile(mU, (1, 4)).astype(ml_dtypes.bfloat16)
    n = L
    tt_ = np.linspace(0.0, 1.0, n, dtype=np.float32)[:, None]
    bands = 16
    w = (2.0 * math.pi * np.arange(n, dtype=np.float32)[:, None] / n).astype(np.float32)
    fr = np.linspace(1e-4, bands - 1, bands, dtype=np.float32)[None, :]
    z = np.concatenate([tt_, np.cos(fr * w), -np.sin(fr * w)], -1).astype(np.float32)
    c["hy_zT"] = np.ascontiguousarray(z.T)
    deltas = np.abs(np.linspace(math.log(1e-2) / 1.5, math.log(1e-2) / 0.3, D, dtype=np.float32))
    c["hy_decay"] = np.exp(-tt_ * deltas[None, :]).astype(np.float32)
    tau = np.arange(n, dtype=np.float64)[:, None]
    kk = (np.arange(NFREQ, dtype=np.float64) + 0.5)[None, :]
    ph = (2.0 * math.pi / 4096.0) * tau * kk
    Cm = np.cos(ph)
    Sm = np.sin(ph)
    def fwd_l(M):
        return np.ascontiguousarray(M.reshape(16, 128, 16, 128).transpose(2, 1, 0, 3)).astype(ml_dtypes.bfloat16)
    c["dft_c"] = fwd_l(Cm)
    c["dft_ms"] = fwd_l(-Sm)
    Gc = (Cm / 2048.0).T.reshape(16, 128, 16, 128)
    Gs = (-Sm / 2048.0).T.reshape(16, 128, 16, 128)
    G = np.concatenate([Gc, Gs], 0)
    c["idft"] = np.ascontiguousarray(G.transpose(2, 1, 0, 3)).astype(ml_dtypes.bfloat16)
    _CONSTS = c
    return c


def build_program(stage=99, dbg=False, stop=None):
    nc = bass.Bass("TRN2", target_bir_lowering=False)
    T = {}

    def din(name, shape, dt=F32):
        T[name] = nc.dram_tensor(name, list(shape), dt, kind="ExternalInput")
        return T[name]

    def dscr(name, shape, dt=F32):
        T[name] = nc.dram_tensor(name, list(shape), dt, kind="Internal")
        return T[name]

    x_in = din("x", [L, D])
    ctx_in = din("ctx", [C, D])
    cT_in = din("cT", [128, 8])
    cctxT_in = din("cctxT", [128, 8])
    mod_w = din("mod_w", [2, D, 6 * D])
    mod_b = din("mod_b", [2, 6 * D])
    ln_g = din("ln_g", [2, 2, D])
    ln_b = din("ln_b", [2, 2, D])
    attn_w_in = din("attn_w_in", [D, 2304])
    attn_lambda = din("attn_lambda", [1, 256])
    attn_subln_g = din("attn_subln_g", [1, 128])
    attn_sink = din("attn_sink", [1, 8])
    attn_w_out = din("attn_w_out", [D, D])
    ident_f_in = din("ident_f", [128, 128])
    ident_b_in = din("ident_b", [128, 128], BF16)
    rotT_in = din("rotT", [128, 128], BF16)
    cosT_in = din("cosT", [128, L])
    sinT_in = din("sinT", [128, L])
    maskL_in = din("maskL", [128, 512], BF16)
    maskU_in = din("maskU", [128, 512], BF16)
    if stage >= 2:
        router_w = din("router_w", [D, NE])
        router_bias = din("router_bias", [1, NE])
        exp_w_gate = din("exp_w_gate", [2, NE, D, D])
        exp_w_up = din("exp_w_up", [2, NE, D, D])
        exp_w_down = din("exp_w_down", [2, NE, D, D])
    if stage >= 3:
        hy_w_in = din("hy_w_in", [D, 3 * D])
        hy_conv_w = din("hy_conv_w", [3, 3 * D])
        hy_conv_b = din("hy_conv_b", [1, 3 * D])
        hy_ffn_w_in = din("hy_ffn_w_in", [33, 64])
        hy_ffn_w_hid = din("hy_ffn_w_hid", [2, 64, 64])
        hy_ffn_bT = din("hy_ffn_bT", [64, 3])
        hy_sin_freqT = din("hy_sin_freqT", [64, 3])
        hy_ffn_w_out = din("hy_ffn_w_out", [64, 4 * D])
        hy_skip = din("hy_skip", [2, D])
        hy_w_out = din("hy_w_out", [D, D])
        hy_zT = din("hy_zT", [33, L])
        hy_decay = din("hy_decay", [L, D])
        dft_c = din("dft_c", [16, 128, 16, 128], BF16)
        dft_ms = din("dft_ms", [16, 128, 16, 128], BF16)
        idft = din("idft", [16, 128, 32, 128], BF16)
        u_scr = dscr("u_scr", [L, 3 * D])
        vb_scr = dscr("vb_scr", [L, D], BF16)
        sd_scr = dscr("sd_scr", [2, L, 2 * D], BF16)
        Hspec = dscr("Hspec", [2, 16, 128, 2 * D])
        z2_scr = dscr("z2_scr", [L, D])
        z2b_scr = dscr("z2b_scr", [L, D], BF16)
    out_t = nc.dram_tensor("out", [L, D], F32, kind="ExternalOutput")
    modrow = dscr("modrow", [2, 6 * D])
    cmodrow = dscr("cmodrow", [1, 2 * D])

    kb = KB(nc)
    kb.init_psum()
    P = kb.es

    ident_f = kb.sb("ident_f_s", [128, 128])
    ident_b = kb.sb("ident_b_s", [128, 128], BF16)
    kb.ld(ident_f[:], ident_f_in.ap(), w=["ident"])
    kb.ld(ident_b[:], ident_b_in.ap(), w=["identb"])
    x = kb.sb("xres", [128, NT, D])
    g_bc = kb.sb("g_bc", [128, D])
    lng_bc = kb.sb("lng_bc", [128, D])
    lnb_bc = kb.sb("lnb_bc", [128, D])
    cols = kb.sb("modcols", [128, 6, 8])
    ccols = kb.sb("cmodcols", [128, 2, 8])
    small = kb.sb("small", [128, 64])
    eps_t = kb.sb("eps_t", [128, 1])
    kb.memset("dve", eps_t[:], LN_EPS, w=["eps"])

    def mod_phase(i, with_ctx):
        with ExitStack() as es:
            sc = kb.sb("silu_c", [128, 8], es=es)
            scc = kb.sb("silu_cc", [128, 8], es=es)
            mb_row = kb.sb("mb_row", [1, 6 * D], es=es)
            orow = kb.sb("orow", [1, 6 * D], es=es)
            corow = kb.sb("corow", [1, 2 * D], es=es)
            wbuf = [kb.sb("modw%d" % j, [128, 8, 512], es=es) for j in range(2)]
            kb.ld(sc[:], cT_in.ap(), w=["silu_c"])
            kb.act(sc[:], sc[:], AF.Silu, r=["silu_c"], w=["silu_c"])
            kb.ld(mb_row[:], mod_b.ap()[i:i + 1, :], w=["mb_row"])
            if with_ctx:
                kb.ld(scc[:], cctxT_in.ap(), w=["silu_cc"])
                kb.act(scc[:], scc[:], AF.Silu, r=["silu_cc"], w=["silu_cc"])
            wv = mod_w.ap()[i].rearrange("(k p) n -> p k n", p=128)
            for n in range(12):
                wb = wbuf[n % 2]
                wk = "modw%d" % (n % 2)
                kb.ld(wb[:], wv[:, :, n * 512:(n + 1) * 512], w=[wk])
                pt, pk = kb.ps()
                for k in range(8):
                    kb.mm(pt[0:1, :], sc[:, k:k + 1], wb[:, k, :], k == 0, k == 7, r=["silu_c", wk], w=[pk])
                kb.tt("dve", orow[0:1, n * 512:(n + 1) * 512], pt[0:1, :], mb_row[0:1, n * 512:(n + 1) * 512], ALU.add,
                      r=[pk, "mb_row"], w=["orow"])
                if with_ctx and n < 4:
                    pt2, pk2 = kb.ps()
                    for k in range(8):
                        kb.mm(pt2[0:1, :], scc[:, k:k + 1], wb[:, k, :], k == 0, k == 7, r=["silu_cc", wk], w=[pk2])
                    kb.tt("dve", corow[0:1, n * 512:(n + 1) * 512], pt2[0:1, :], mb_row[0:1, n * 512:(n + 1) * 512], ALU.add,
                          r=[pk2, "mb_row"], w=["corow"])
            kb.ld(modrow.ap()[i:i + 1, :], orow[:], r=["orow"], w=["modrow%d" % i])
            if with_ctx:
                kb.ld(cmodrow.ap(), corow[:], r=["corow"], w=["cmodrow"])
            for j in (0, 1, 3, 4):
                kb.ld(cols[:, j, :], col_view(modrow, i * 6 * D + j * D, 8), r=["modrow%d" % i], w=["cols%d" % j], slow=True)
            for j in (1, 4):
                kb.ts("dve", cols[:, j, :], cols[:, j, :], 1.0, None, ALU.add, None, r=["cols%d" % j], w=["cols%d" % j])
            if with_ctx:
                for j in (0, 1):
                    kb.ld(ccols[:, j, :], col_view(cmodrow, j * D, 8), r=["cmodrow"], w=["ccols%d" % j], slow=True)
                kb.ts("dve", ccols[:, 1, :], ccols[:, 1, :], 1.0, None, ALU.add, None, r=["ccols1"], w=["ccols1"])
        kb.barrier()

    def load_gate_ln(i, which):
        goff = i * 6 * D + (2 if which == 0 else 5) * D
        kb.ld(g_bc[:], bc_row(modrow, goff, D), r=["modrow%d" % i], w=["g_bc"])
        kb.ld(lng_bc[:], bc_row(ln_g, (i * 2 + which) * D, D), w=["lng_bc"])
        kb.ld(lnb_bc[:], bc_row(ln_b, (i * 2 + which) * D, D), w=["lnb_bc"])

    def ln_inplace(t, es_tmp):
        junk, st = es_tmp
        xt = x[:, t, :]
        xk = "x%d" % t
        kb.memset("dve", st[:, 0:2], 0.0, w=["lnst"])
        kb.act(junk[:], xt, AF.Identity, r=[xk, "lnst"], w=["tmp_o0", "tmp_o1", "lnst_a"], accum_out=st[:, 0:1])
        kb.act(junk[:], xt, AF.Square, r=[xk, "lnst"], w=["tmp_o0", "tmp_o1", "lnst_b"], accum_out=st[:, 1:2])
        kb.ts("dve", st[:, 2:4], st[:, 0:2], 1.0 / D, None, ALU.mult, None, r=["lnst_a", "lnst_b", "lnst"], w=["lnst2"])
        kb.tt("dve", st[:, 4:5], st[:, 2:3], st[:, 2:3], ALU.mult, r=["lnst2"], w=["lnst3"])
        kb.tt("dve", st[:, 5:6], st[:, 3:4], st[:, 4:5], ALU.subtract, r=["lnst2", "lnst3"], w=["lnst4"])
        kb.act(st[:, 6:7], st[:, 5:6], AF.Sqrt, r=["lnst4", "eps"], w=["lnst5"], bias=eps_t[:, 0:1])
        kb.op("dve", lambda e: e.reciprocal(out=st[:, 7:8], in_=st[:, 6:7]), r=["lnst5"], w=["lnst6"])
        kb.ts("dve", xt, xt, st[:, 2:3], st[:, 7:8], ALU.subtract, ALU.mult, r=[xk, "lnst2", "lnst6"], w=[xk])
        kb.tt("dve", xt, xt, lng_bc[:], ALU.mult, r=[xk, "lng_bc"], w=[xk])
        kb.tt("dve", xt, xt, lnb_bc[:], ALU.add, r=[xk, "lnb_bc"], w=[xk])

    def attention_layer():
        i = 0
        lam_init = 0.8 - 0.6 * math.exp(-0.3 * i)
        xflat = x.bitcast(BF16)[:].rearrange("p a b -> p (a b)")
        hT = xflat[:, 0:8 * (L + C)].rearrange("p (k n) -> p k n", k=8)
        xin_f = x[:, 10:14, :]
        with ExitStack() as es:
            qaT = kb.sb("qaT", [128, 4, L], BF16, es=es)
            kaT = kb.sb("kaT", [128, 4, L + C], BF16, es=es)
            qwT = kb.sb("qwT", [128, 4, L], BF16, es=es)
            kwT = kb.sb("kwT", [128, 2, L + C], BF16, es=es)
            va = kb.sb("va", [128, 18, 4, 130], BF16, es=es)
            vw = kb.sb("vw", [128, 18, 2, 66], BF16, es=es)
            rotT = kb.sb("rotT_s", [128, 128], BF16, es=es)
            kb.ld(rotT[:], rotT_in.ap(), w=["rotT"])
            kb.memset("pool", va[:].rearrange("p a b c -> p (a b c)"), 1.0, w=["va%d" % t for t in range(18)])
            kb.memset("pool", vw[:].rearrange("p a b c -> p (a b c)"), 1.0, w=["vw%d" % t for t in range(18)])
            with ExitStack() as es2:
                cs = [kb.sb("cs%d" % j, [128, 2, 512], es=es2) for j in range(2)]
                for g in range(5):
                    xb = xin_f
                    xk = "xin"
                    nt = 4 if g < 4 else 2
                    if g < 4:
                        kb.ld(xb[:, :, :], x_in.ap()[g * 512:(g + 1) * 512, :].rearrange("(t p) d -> p t d", p=128), w=[xk])
                    else:
                        kb.ld(xb[:, 0:2, :], ctx_in.ap().rearrange("(t p) d -> p t d", p=128), w=[xk])
                    ccol = cols if g < 4 else ccols
                    ck = ["cols0", "cols1"] if g < 4 else ["ccols0", "ccols1"]
                    for k in range(8):
                        pt, pk = kb.ps()
                        for j in range(nt):
                            kb.tr(pt[:, j * 128:(j + 1) * 128], xb[:, j, k * 128:(k + 1) * 128], ident_f[:], r=[xk], w=[pk])
                        kb.act(hT[:, k, g * 512:g * 512 + nt * 128], pt[:, 0:nt * 128], AF.Identity, r=[pk] + ck, w=["hT%d_%d" % (k, g)],
                               bias=ccol[:, 0, k:k + 1], scale=ccol[:, 1, k:k + 1])
                if stop == "A":
                    return
                wst = [kb.sb("wst%d" % j, [128, 8, 128], BF16, es=es2) for j in range(3)]
                wstf = [kb.sb("wstf%d" % j, [128, 8, 128], F32, es=es2) for j in range(2)]
                qb = [kb.sb("qb%d" % j, [128, 512], BF16, es=es2) for j in range(2)]
                t1 = [kb.sb("rt1_%d" % j, [128, 512], es=es2) for j in range(2)]
                t2 = [kb.sb("rt2_%d" % j, [128, 512], es=es2) for j in range(2)]
                w_in_v = attn_w_in.ap().rearrange("(k p) n -> p k n", p=128)
                jobs = []
                for h in range(4):
                    jobs.append(("qa", h, [(h * 128, 128)], qaT, False))
                for h in range(4):
                    jobs.append(("qw", h, [(512 + h * 128, 128)], qwT, False))
                for h in range(4):
                    jobs.append(("ka", h, [(1024 + h * 128, 128)], kaT, True))
                for g in range(2):
                    jobs.append(("kw", g, [(2048 + g * 64, 64), (2048 + g * 64, 64)], kwT, True))
                cnt = 0
                for cch in range(5):
                    n0 = cch * 512
                    nn = 512 if cch < 4 else 256
                    if cch < 4:
                        csb = cs[cch % 2]
                        csk = "cs%d" % (cch % 2)
                        kb.ld(csb[:, 0, :], cosT_in.ap()[:, n0:n0 + 512], w=[csk])
                        kb.ld(csb[:, 1, :], sinT_in.ap()[:, n0:n0 + 512], w=[csk])
                    for ji, (nm, idx, colspec, dest, has_ctx) in enumerate(jobs):
                        if cch == 4 and not has_ctx:
                            continue
                        ws = wst[cnt % 3]
                        wk = "wst%d" % (cnt % 3)
                        o = 0
                        wf = wstf[cnt % 2]
                        wfk = "wstf%d" % (cnt % 2)
                        for (c0, cn) in colspec:
                            kb.ld(wf[:, :, o:o + cn], w_in_v[:, :, c0:c0 + cn], w=[wfk])
                            o += cn
                        kb.cp("pool", ws[:], wf[:], r=[wfk], w=[wk])
                        pa, pak = kb.ps()
                        for k in range(8):
                            kb.mm(pa[:, 0:nn], ws[:, k, :], hT[:, k, n0:n0 + nn], k == 0, k == 7, r=[wk, "hT%d_%d" % (k, cch)], w=[pak])
                        dk = "%s%d_%d" % (nm, idx, cch)
                        b = cnt % 2
                        cnt += 1
                        if stop == "B1":
                            kb.cp("act", qb[b][:, 0:nn], pa[:, 0:nn], r=[pak], w=["qb%d" % b])
                            continue
                        if stop in ("B2", "B2a", "B2b"):
                            if cch == 4:
                                continue
                            kb.cp("act", qb[b][:], pa[:], r=[pak], w=["qb%d" % b])
                            if stop != "B2b":
                                pb, pbk = kb.ps()
                                kb.mm(pb[:], rotT[:], qb[b][:], True, True, r=["rotT", "qb%d" % b], w=[pbk])
                            if stop != "B2a":
                                kb.tt("dve", t1[b][:], pa[:], csb[:, 0, :], ALU.mult, r=[pak, csk, "qb%d" % b], w=["rt1_%d" % b])
                            if stop == "B2":
                                kb.tt("dve", t2[b][:], pb[:], csb[:, 1, :], ALU.mult, r=[pbk, csk], w=["rt2_%d" % b])
                            continue
                        if cch == 4:
                            kb.cp("act", dest[:, idx, n0:n0 + nn], pa[:, 0:nn], r=[pak], w=[dk])
                            continue
                        kb.cp("act", qb[b][:], pa[:], r=[pak], w=["qb%d" % b])
                        pb, pbk = kb.ps()
                        kb.mm(pb[:], rotT[:], qb[b][:], True, True, r=["rotT", "qb%d" % b], w=[pbk])
                        kb.tt("dve", t1[b][:], pa[:], csb[:, 0, :], ALU.mult, r=[pak, csk], w=["rt1_%d" % b])
                        kb.tt("dve", t2[b][:], pb[:], csb[:, 1, :], ALU.mult, r=[pbk, csk], w=["rt2_%d" % b])
                        kb.tt("dve", dest[:, idx, n0:n0 + 512], t1[b][:], t2[b][:], ALU.add, r=["rt1_%d" % b, "rt2_%d" % b], w=[dk])
                if stop in ("B", "B1", "B2", "B2a", "B2b"):
                    return
                wv = kb.sb("wv", [128, 8, 640], BF16, es=es2)
                for (d0, c0, cn) in ((0, 1536, 128), (128, 1664, 128), (256, 1792, 128), (384, 1920, 128), (512, 2176, 128)):
                    wf = wstf[cnt % 2]
                    wfk = "wstf%d" % (cnt % 2)
                    cnt += 1
                    kb.ld(wf[:], w_in_v[:, :, c0:c0 + cn], w=[wfk])
                    kb.cp("pool", wv[:, :, d0:d0 + cn], wf[:], r=[wfk], w=["wv"])
                for t in range(18):
                    g = t // 4
                    p1, p1k = kb.ps()
                    p2, p2k = kb.ps()
                    for k in range(8):
                        kb.mm(p1[:], hT[:, k, t * 128:(t + 1) * 128], wv[:, k, 0:512], k == 0, k == 7, r=["wv", "hT%d_%d" % (k, g)], w=[p1k])
                    for k in range(8):
                        kb.mm(p2[:, 0:128], hT[:, k, t * 128:(t + 1) * 128], wv[:, k, 512:640], k == 0, k == 7, r=["wv", "hT%d_%d" % (k, g)], w=[p2k])
                    kb.cp("act" if t % 2 == 0 else "dve", va[:, t, :, 0:128], p1[:].rearrange("p (h d) -> p h d", h=4), r=[p1k], w=["va%d" % t])
                    kb.cp("dve" if t % 2 == 0 else "act", vw[:, t, :, 0:64], p2[:, 0:128].rearrange("p (h d) -> p h d", h=2), r=[p2k], w=["vw%d" % t])
            kb.barrier()
            if stop == "V":
                return
            with ExitStack() as es3:
                catT = kb.sb("catT", [128, 8, 512], BF16, es=es3)
                tmpw = x[:, 14:16, :].rearrange("p a (k n) -> p (a k) n", k=4)
                w_out_b = kb.sb("w_out_b", [128, 8, D], BF16, es=es3)
                w_out_v = attn_w_out.ap().rearrange("(k p) n -> p k n", p=128)
                for cc in range(4):
                    kb.ld(tmpw, w_out_v[:, :, cc * 256:(cc + 1) * 256], w=["x14", "x15"])
                    kb.cp("pool", w_out_b[:, :, cc * 256:(cc + 1) * 256], tmpw, r=["x14", "x15"], w=["w_out_b"])
                if stop == "C0a":
                    return
                lamt = kb.sb("lamt", [128, 256], es=es3)
                lam2 = kb.sb("lam2", [128, 16], es=es3)
                kb.ld(lamt[:], bc_row(attn_lambda, 0, 256), w=["lamt"])
                kb.tt("dve", lamt[:, 0:64], lamt[:, 0:64], lamt[:, 64:128], ALU.mult, r=["lamt"], w=["lamt"])
                kb.tt("dve", lamt[:, 128:192], lamt[:, 128:192], lamt[:, 192:256], ALU.mult, r=["lamt"], w=["lamt"])
                kb.memset("dve", lam2[:], 0.0, w=["lam2"])
                kb.act(lamt[:, 64:128], lamt[:, 0:64], AF.Identity, r=["lamt", "lam2"], w=["lamt", "lam2a"], accum_out=lam2[:, 0:1])
                kb.act(lamt[:, 192:256], lamt[:, 128:192], AF.Identity, r=["lamt", "lam2"], w=["lamt", "lam2b"], accum_out=lam2[:, 1:2])
                kb.act(lam2[:, 2:4], lam2[:, 0:2], AF.Exp, r=["lam2a", "lam2b"], w=["lam2c"])
                kb.tt("dve", lam2[:, 4:5], lam2[:, 3:4], lam2[:, 2:3], ALU.subtract, r=["lam2c"], w=["lam2d"])
                kb.ts("dve", lam2[:, 5:6], lam2[:, 4:5], -lam_init, None, ALU.add, None, r=["lam2d"], w=["neglam"])
                if stop == "C0b":
                    return
                sg = kb.sb("sg", [128, 1], es=es3)
                kb.ld(sg[:], bass.AP(attn_subln_g, 0, [[1, 128], [1, 1]]), w=["sg"], slow=True)
                kb.ts("dve", sg[:], sg[:], 1.0 - lam_init, None, ALU.mult, None, r=["sg"], w=["sg"])
                sk = kb.sb("sinkexp", [128, 8], es=es3)
                kb.ld(sk[:], bc_row(attn_sink, 0, 8), w=["sink"])
                kb.act(sk[:], sk[:], AF.Exp, r=["sink"], w=["sink"])
                if stop == "C0c":
                    return
                maskL = kb.sb("maskL_s", [128, 512], BF16, es=es3)
                maskU = kb.sb("maskU_s", [128, 512], BF16, es=es3)
                kb.ld(maskL[:], maskL_in.ap(), w=["maskL"])
                kb.ld(maskU[:], maskU_in.ap(), w=["maskU"])
                ptb = [kb.sb("ptb%d" % j, [128, 512], BF16, es=es3) for j in range(4)]
                o0n = kb.sb("o0n", [128, 4, 128], es=es3)
                oa = kb.sb("oa", [128, 4, 128], es=es3)
                junk = kb.sb("junk_a", [128, 128], es=es3)
                st = kb.sb("st_a", [128, 4, 8], es=es3)
                owb = kb.sb("owb", [128, 512], es=es3)
                tmp = kb.sb("tmp_o", [128, D], es=es3)
                st2 = kb.sb("st_ln", [128, 8], es=es3)
                load_gate_ln(0, 0)
                pcnt = 0
                if stop == "C0":
                    return
                for qc in range(4):
                    for h in range(4):
                        if stop == "C1" and h == 1:
                            return
                        if stop in ("W1", "W1a", "W1b", "W1c", "W2", "W3"):
                            break
                        for m in range(2):
                            accs = kb.ps_hold(4)
                            for kt in range(18):
                                ps_s, psk = kb.ps()
                                kch = kt // 4
                                kb.mm(ps_s[:], kaT[m * 64:(m + 1) * 64, h, kt * 128:(kt + 1) * 128], qaT[m * 64:(m + 1) * 64, h, qc * 512:(qc + 1) * 512],
                                      True, True, r=["ka%d_%d" % (h, kch), "qa%d_%d" % (h, qc)], w=[psk])
                                pb_ = ptb[pcnt % 4]
                                pbk = "ptb%d" % (pcnt % 4)
                                pcnt += 1
                                kb.act(pb_[:], ps_s[:], AF.Exp, r=[psk], w=[pbk], scale=0.125)
                                for s in range(4):
                                    kb.mm(accs[s][0][:, 0:129], pb_[:, s * 128:(s + 1) * 128], va[:, kt, h, 0:129], kt == 0, kt == 17,
                                          r=[pbk, "va%d" % kt], w=[accs[s][1]])
                            for s in range(4):
                                ac, ack = accs[s]
                                sk_ = "st_a%d" % s
                                kb.op("dve", lambda e, ac=ac, s=s: e.reciprocal(out=st[:, s, 0:1], in_=ac[:, 128:129]), r=[ack], w=[sk_])
                                if m == 0:
                                    kb.ts("dve", o0n[:, s, :], ac[:, 0:128], st[:, s, 0:1], None, ALU.mult, None, r=[ack, sk_], w=["o0n%d" % s])
                                else:
                                    kb.tt("dve", st[:, s, 1:2], st[:, s, 0:1], lam2[:, 5:6], ALU.mult, r=[sk_, "neglam"], w=[sk_])
                                    kb.stt("dve", oa[:, s, :], ac[:, 0:128], st[:, s, 1:2], o0n[:, s, :], ALU.mult, ALU.add,
                                           r=[ack, sk_, "o0n%d" % s], w=["oa%d" % s])
                                    kb.memset("dve", st[:, s, 2:3], 0.0, w=[sk_])
                                    kb.act(junk[:], oa[:, s, :], AF.Square, r=["oa%d" % s, sk_], w=["junk_a", sk_], accum_out=st[:, s, 2:3])
                                    kb.act(st[:, s, 3:4], st[:, s, 2:3], AF.Sqrt, r=[sk_, "eps"], w=[sk_], bias=eps_t[:, 0:1], scale=1.0 / 128)
                                    kb.op("dve", lambda e, s=s: e.reciprocal(out=st[:, s, 4:5], in_=st[:, s, 3:4]), r=[sk_], w=[sk_])
                                    kb.ts("dve", oa[:, s, :], oa[:, s, :], st[:, s, 4:5], None, ALU.mult, None, r=["oa%d" % s, sk_], w=["oa%d" % s])
                            kb.ps_release(accs)
                            if m == 1:
                                ptr, ptrk = kb.ps()
                                for s in range(4):
                                    kb.tr(ptr[:, s * 128:(s + 1) * 128], oa[:, s, :], ident_f[:], r=["oa%d" % s], w=[ptrk])
                                kb.act(catT[:, h, :], ptr[:], AF.Identity, r=[ptrk, "sg"], w=["cat%d" % h], scale=sg[:, 0:1])
                    for nl in range(4):
                        n = 4 * qc + nl
                        if stop in ("C2", "W3") and nl == 2:
                            return
                        for g in range(2):
                            kts = []
                            if n >= 1:
                                kts.append((n - 1, "L"))
                            kts.append((n, None))
                            if n <= 14:
                                kts.append((n + 1, "U"))
                            kts.append((16, None))
                            kts.append((17, None))
                            accs = kb.ps_hold(4)
                            for ki, (kt, mt) in enumerate(kts):
                                kch = kt // 4
                                pb_ = ptb[pcnt % 4]
                                pbk = "ptb%d" % (pcnt % 4)
                                pcnt += 1
                                for p in range(2):
                                    ps_s, psk = kb.ps()
                                    for jj in range(2):
                                        j = 2 * jj + p
                                        head = 4 * g + j
                                        ti = head // 2
                                        kb.mm(ps_s[:, jj * 128:(jj + 1) * 128], kwT[p * 64:(p + 1) * 64, g, kt * 128:(kt + 1) * 128],
                                              qwT[p * 64:(p + 1) * 64, ti, n * 128:(n + 1) * 128], True, True,
                                              r=["kw%d_%d" % (g, kch), "qw%d_%d" % (ti, qc)], w=[psk])
                                    kb.act(pb_[:, p * 256:(p + 1) * 256], ps_s[:, 0:256], AF.Exp, r=[psk], w=[pbk], scale=0.125)
                                if mt is not None and stop != "W1a":
                                    mk = maskL if mt == "L" else maskU
                                    kb.tt("dve", pb_[:], pb_[:], mk[:], ALU.mult, r=[pbk, "maskL", "maskU"], w=[pbk])
                                if stop == "W1b":
                                    continue
                                for j in range(4):
                                    sl = (j % 2) * 2 + j // 2
                                    kb.mm(accs[j][0][:, 0:65], pb_[:, sl * 128:(sl + 1) * 128], vw[:, kt, g, 0:65], ki == 0, ki == len(kts) - 1,
                                          r=[pbk, "vw%d" % kt], w=[accs[j][1]])
                            for j in range(4):
                                if stop in ("W1b", "W1c"):
                                    continue
                                head = 4 * g + j
                                ac, ack = accs[j]
                                sk_ = "st_a%d" % j
                                kb.tt("dve", st[:, j, 0:1], ac[:, 64:65], sk[:, head:head + 1], ALU.add, r=[ack, "sink"], w=[sk_])
                                kb.op("dve", lambda e, j=j: e.reciprocal(out=st[:, j, 1:2], in_=st[:, j, 0:1]), r=[sk_], w=[sk_])
                                kb.ts("dve", owb[:, head * 64:(head + 1) * 64], ac[:, 0:64], st[:, j, 1:2], None, ALU.mult, None, r=[ack, sk_], w=["owb%d" % head])
                            kb.ps_release(accs)
                            if stop in ("W1", "W1a", "W1b", "W1c"):
                                return
                        if stop == "W2":
                            return
                        ptr, ptrk = kb.ps()
                        for tI in range(4):
                            kb.tr(ptr[:, tI * 128:(tI + 1) * 128], owb[:, tI * 128:(tI + 1) * 128], ident_f[:], r=["owb%d" % (2 * tI), "owb%d" % (2 * tI + 1)], w=[ptrk])
                        kb.cp("act", catT[:, 4:8, nl * 128:(nl + 1) * 128], ptr[:].rearrange("p (t q) -> p t q", t=4), r=[ptrk], w=["catw%d" % nl])
                    if stop == "C3" and qc == 1:
                        return
                    for nl in range(4):
                        t = 4 * qc + nl
                        xk = "x%d" % t
                        kb.ld(x[:, t, :], x_in.ap()[t * 128:(t + 1) * 128, :], w=[xk])
                        rk = ["cat%d" % h for h in range(4)] + ["catw%d" % nl, "w_out_b"]
                        for cchunk in range(2):
                            py, pyk = kb.ps()
                            for k in range(8):
                                kb.mm(py[:], catT[:, k, nl * 128:(nl + 1) * 128], w_out_b[:, k, cchunk * 512:(cchunk + 1) * 512], k == 0, k == 7, r=rk, w=[pyk])
                            kb.tt("dve", tmp[:, cchunk * 512:(cchunk + 1) * 512], py[:], g_bc[:, cchunk * 512:(cchunk + 1) * 512], ALU.mult,
                                  r=[pyk, "g_bc"], w=["tmp_o%d" % cchunk])
                        kb.stt("dve", x[:, t, :], x[:, t, :], ALPHA, tmp[:], ALU.mult, ALU.add, r=[xk, "tmp_o0", "tmp_o1"], w=[xk, "tmp_o0", "tmp_o1"])
                        ln_inplace(t, (tmp, st2))
            kb.barrier()

    def bl(ap2, n):
        a = ap2.ap
        return bass.AP(ap2.tensor, ap2.offset, [list(a[0]), list(a[1]), [0, n]])

    def bm(ap2, n):
        a = ap2.ap
        return bass.AP(ap2.tensor, ap2.offset, [list(a[0]), [0, n], list(a[1])])

    def moe_layer(i):
        load_gate_ln(i, 1)
        BIG = 1.0e9
        with ExitStack() as es:
            xmT = kb.sb("xmT", [128, 8, L], BF16, es=es)
            wts = kb.sb("wts", [128, NT, NE], es=es)
            with ExitStack() as esA:
                xmf = kb.sb("xmf", [128, 8, 512], es=esA)
                rw = kb.sb("rw", [128, 8, NE], es=esA)
                rb = kb.sb("rb_bc", [128, NE], es=esA)
                lg = kb.sb("lg", [128, NT, NE], es=esA)
                sc_ = kb.sb("rsc", [128, NT, NE], es=esA)
                sel = kb.sb("rsel", [128, NT, NE], es=esA)
                m1t = kb.sb("rm1", [128, NT, NE], es=esA)
                m2t = kb.sb("rm2", [128, NT, NE], es=esA)
                r16 = kb.sb("r16", [128, 8, NT], es=esA)
                g64 = kb.sb("g64", [128, 10, 64], es=esA)
                kb.ld(rw[:], router_w.ap().rearrange("(k p) e -> p k e", p=128), w=["rw"])
                kb.ld(rb[:], bc_row(router_bias, 0, NE), w=["rb"])
                for g in range(4):
                    for k in range(8):
                        pt, pk = kb.ps()
                        for j in range(4):
                            kb.tr(pt[:, j * 128:(j + 1) * 128], x[:, 4 * g + j, k * 128:(k + 1) * 128], ident_f[:], r=["x%d" % (4 * g + j)], w=[pk])
                        kb.act(xmT[:, k, g * 512:(g + 1) * 512], pt[:], AF.Identity, r=[pk, "cols3", "cols4"], w=["xmT%d_%d" % (k, g)],
                               bias=cols[:, 3, k:k + 1], scale=cols[:, 4, k:k + 1])
                        kb.ts("dve", xmf[:, k, :], pt[:], cols[:, 4, k:k + 1], cols[:, 3, k:k + 1], ALU.mult, ALU.add, r=[pk, "cols3", "cols4"], w=["xmf%d" % k])
                    for j in range(4):
                        pl, plk = kb.ps()
                        for k in range(8):
                            kb.mm(pl[:, 0:NE], xmf[:, k, j * 128:(j + 1) * 128], rw[:, k, :], k == 0, k == 7, r=["xmf%d" % k, "rw"], w=[plk])
                        kb.cp("dve", lg[:, 4 * g + j, :], pl[:, 0:NE], r=[plk], w=["lg"])
                R = lambda *k: list(k)
                mx = r16[:, 0, :]
                kb.op("dve", lambda e: e.tensor_reduce(out=mx, in_=lg[:], axis=mybir.AxisListType.X, op=ALU.max), r=["lg"], w=["r16_0"])
                kb.tt("dve", sc_[:], lg[:], bl(mx, NE), ALU.subtract, r=["lg", "r16_0"], w=["rsc"])
                kb.act(sc_[:], sc_[:], AF.Exp, r=["rsc"], w=["rsc"])
                ssum = r16[:, 1, :]
                kb.op("dve", lambda e: e.tensor_reduce(out=ssum, in_=sc_[:], axis=mybir.AxisListType.X, op=ALU.add), r=["rsc"], w=["r16_1"])
                kb.op("dve", lambda e: e.reciprocal(out=r16[:, 2, :], in_=ssum), r=["r16_1"], w=["r16_2"])
                kb.tt("dve", sc_[:], sc_[:], bl(r16[:, 2, :], NE), ALU.mult, r=["rsc", "r16_2"], w=["rsc"])
                kb.tt("dve", sel[:], sc_[:], bm(rb[:], NT), ALU.add, r=["rsc", "rb"], w=["rsel"])
                selflat = sel[:].rearrange("p t e -> p (t e)")

                def gview(j):
                    a = selflat.ap
                    return bass.AP(selflat.tensor, selflat.offset + j, [list(a[0]), [4, 64]])
                hi1, lo1, hi2, lo2, top1, mn, mxl, sec, gs, gm = [g64[:, q, :] for q in range(10)]
                kb.tt("dve", hi1, gview(0), gview(1), ALU.max, r=["rsel"], w=["g0"])
                kb.tt("dve", lo1, gview(0), gview(1), ALU.min, r=["rsel"], w=["g1"])
                kb.tt("dve", hi2, gview(2), gview(3), ALU.max, r=["rsel"], w=["g2"])
                kb.tt("dve", lo2, gview(2), gview(3), ALU.min, r=["rsel"], w=["g3"])
                kb.tt("dve", top1, hi1, hi2, ALU.max, r=["g0", "g2"], w=["g4"])
                kb.tt("dve", mn, hi1, hi2, ALU.min, r=["g0", "g2"], w=["g5"])
                kb.tt("dve", mxl, lo1, lo2, ALU.max, r=["g1", "g3"], w=["g6"])
                kb.tt("dve", sec, mn, mxl, ALU.max, r=["g5", "g6"], w=["g7"])
                kb.tt("dve", gs, top1, sec, ALU.add, r=["g4", "g7"], w=["g8"])
                gmax = r16[:, 3, :]
                kb.op("dve", lambda e: e.tensor_reduce(out=gmax, in_=gs.rearrange("p (t g) -> p t g", g=4), axis=mybir.AxisListType.X, op=ALU.max), r=["g8"], w=["r16_3"])
                kb.tt("dve", gm.rearrange("p (t g) -> p t g", g=4), gs.rearrange("p (t g) -> p t g", g=4), bl(gmax, 4), ALU.is_equal, r=["g8", "r16_3"], w=["g9"])
                kb.ts("dve", gm, gm, -1.0, BIG, ALU.add, ALU.mult, r=["g9"], w=["g9"])
                kb.tt("dve", m1t[:].rearrange("p t (g q) -> p (t g) q", q=4), sel[:].rearrange("p t (g q) -> p (t g) q", q=4), bl(gm, 4), ALU.add,
                      r=["rsel", "g9"], w=["rm1"])
                mx1 = r16[:, 4, :]
                kb.op("dve", lambda e: e.tensor_reduce(out=mx1, in_=m1t[:], axis=mybir.AxisListType.X, op=ALU.max), r=["rm1"], w=["r16_4"])
                kb.tt("dve", m2t[:], m1t[:], bl(mx1, NE), ALU.is_equal, r=["rm1", "r16_4"], w=["rm2"])
                kb.stt("dve", m1t[:], m2t[:], -BIG, m1t[:], ALU.mult, ALU.add, r=["rm1", "rm2"], w=["rm1"])
                mx2 = r16[:, 5, :]
                kb.op("dve", lambda e: e.tensor_reduce(out=mx2, in_=m1t[:], axis=mybir.AxisListType.X, op=ALU.max), r=["rm1"], w=["r16_5"])
                kb.tt("dve", m1t[:], m1t[:], bl(mx2, NE), ALU.is_equal, r=["rm1", "r16_5"], w=["rm1"])
                kb.tt("dve", m2t[:], m2t[:], m1t[:], ALU.add, r=["rm1", "rm2"], w=["rm2"])
                kb.tt("dve", m2t[:], m2t[:], sc_[:], ALU.mult, r=["rm2", "rsc"], w=["rm2"])
                ws_ = r16[:, 6, :]
                kb.op("dve", lambda e: e.tensor_reduce(out=ws_, in_=m2t[:], axis=mybir.AxisListType.X, op=ALU.add), r=["rm2"], w=["r16_6"])
                kb.op("dve", lambda e: e.reciprocal(out=r16[:, 7, :], in_=ws_), r=["r16_6"], w=["r16_7"])
                kb.tt("dve", wts[:], m2t[:], bl(r16[:, 7, :], NE), ALU.mult, r=["rm2", "r16_7"], w=["wts"])
                for t in range(NT):
                    kb.ts("dve", x[:, t, :], x[:, t, :], ALPHA, None, ALU.mult, None, r=["x%d" % t], w=["x%d" % t])
            kb.barrier()
            if stop == "route":
                kb.cp("dve", x[:, 0, 0:256], wts[:].rearrange("p t e -> p (t e)"), r=["wts"], w=["x0"])
                return
            with ExitStack() as esB:
                NP = 8
                wp = [kb.sb("ewp%d" % j, [128, 8, 128], BF16, es=esB) for j in range(NP)]
                Wdb = [kb.sb("ewd%d" % j, [128, 8, D], BF16, es=esB) for j in range(2)]
                stg = [kb.sb("estg%d" % j, [128, 8, 128], es=esB) for j in range(2)]
                HTt = kb.sb("HTt", [128, 8, L], BF16, es=esB)
                sgt = [kb.sb("sgt%d" % j, [128, 512], es=esB) for j in range(2)]
                pcn = 0
                scnt = 0
                ceng = ("pool", "act", "pool", "dve")

                def stage_piece(src_view, pc, dst_ap, dst_key, scale_g):
                    nonlocal scnt
                    sg_ = stg[scnt % 2]
                    sgk = "estg%d" % (scnt % 2)
                    eng = ceng[scnt % 4]
                    scnt += 1
                    kb.ld(sg_[:], src_view[:, :, pc * 128:(pc + 1) * 128], w=[sgk])
                    if scale_g:
                        kb.tt("pool" if eng == "act" else eng, dst_ap, sg_[:], bm(g_bc[:, pc * 128:(pc + 1) * 128], 8), ALU.mult, r=[sgk, "g_bc"], w=[dst_key])
                    else:
                        kb.cp(eng, dst_ap, sg_[:], r=[sgk], w=[dst_key])

                for e_ in range(NE):
                    gv = exp_w_gate.ap()[i, e_].rearrange("(k p) n -> p k n", p=128)
                    uv = exp_w_up.ap()[i, e_].rearrange("(k p) n -> p k n", p=128)
                    dv = exp_w_down.ap()[i, e_].rearrange("(k p) n -> p k n", p=128)
                    Wd = Wdb[e_ % 2]
                    Wdk = "ewd%d" % (e_ % 2)
                    for pc in range(8):
                        stage_piece(dv, pc, Wd[:, :, pc * 128:(pc + 1) * 128], Wdk, True)
                    for f in range(8):
                        wg_ = wp[pcn % NP]
                        wgk = "ewp%d" % (pcn % NP)
                        pcn += 1
                        wu_ = wp[pcn % NP]
                        wuk = "ewp%d" % (pcn % NP)
                        pcn += 1
                        stage_piece(gv, f, wg_[:], wgk, False)
                        stage_piece(uv, f, wu_[:], wuk, False)
                        for tc in range(4):
                            pg, pgk = kb.ps()
                            pu, puk = kb.ps()
                            for k in range(8):
                                kb.mm(pg[:], wg_[:, k, :], xmT[:, k, tc * 512:(tc + 1) * 512], k == 0, k == 7, r=[wgk, "xmT%d_%d" % (k, tc)], w=[pgk])
                            for k in range(8):
                                kb.mm(pu[:], wu_[:, k, :], xmT[:, k, tc * 512:(tc + 1) * 512], k == 0, k == 7, r=[wuk, "xmT%d_%d" % (k, tc)], w=[puk])
                            st_ = sgt[tc % 2]
                            stk = "sgt%d" % (tc % 2)
                            kb.act(st_[:], pg[:], AF.Silu, r=[pgk], w=[stk])
                            kb.tt("dve", HTt[:, f, tc * 512:(tc + 1) * 512], st_[:], pu[:], ALU.mult, r=[stk, puk], w=["HT%d_%d" % (f, tc)])
                    for tc in range(4):
                        for j in range(4):
                            t = 4 * tc + j
                            for c in range(2):
                                py, pyk = kb.ps()
                                for f in range(8):
                                    kb.mm(py[:], HTt[:, f, t * 128:(t + 1) * 128], Wd[:, f, c * 512:(c + 1) * 512], f == 0, f == 7, r=["HT%d_%d" % (f, tc), Wdk], w=[pyk])
                                kb.stt("dve", x[:, t, c * 512:(c + 1) * 512], py[:], wts[:, t, e_:e_ + 1], x[:, t, c * 512:(c + 1) * 512], ALU.mult, ALU.add,
                                       r=[pyk, "wts", "x%d" % t], w=["x%d" % t])
            kb.barrier()
            with ExitStack() as esC:
                junkL = kb.sb("junkL", [128, D], es=esC)
                stL = kb.sb("stL", [128, 8], es=esC)
                for t in range(NT):
                    ln_inplace(t, (junkL, stL))
        kb.barrier()

    def hyena_layer():
        i = 1
        PI = math.pi
        with ExitStack() as es:
            hT = kb.sb("hTp", [128, 8, L + 2], BF16, es=es)
            kb.memset("pool", hT[:, :, 0:1], 0.0, w=["hTp_l"])
            kb.memset("pool", hT[:, :, L + 1:L + 2], 0.0, w=["hTp_r"])
            for g in range(4):
                for k in range(8):
                    pt, pk = kb.ps()
                    for j in range(4):
                        kb.tr(pt[:, j * 128:(j + 1) * 128], x[:, 4 * g + j, k * 128:(k + 1) * 128], ident_f[:], r=["x%d" % (4 * g + j)], w=[pk])
                    kb.act(hT[:, k, 1 + g * 512:1 + (g + 1) * 512], pt[:], AF.Identity, r=[pk, "cols0", "cols1"], w=["hTp%d_%d" % (k, g)],
                           bias=cols[:, 0, k:k + 1], scale=cols[:, 1, k:k + 1])
            hkeys = ["hTp_l", "hTp_r"] + ["hTp%d_%d" % (k, g) for k in range(8) for g in range(4)]
            Wj2 = [[kb.sb("hyW%d_%d" % (j, q), [128, 8, 512], BF16, es=es) for j in range(3)] for q in range(2)]
            cw2 = [kb.sb("hycw%d" % q, [128, 4, 512], es=es) for q in range(2)]
            stg = [kb.sb("hystg%d" % j, [128, 8, 128], es=es) for j in range(2)]
            ev = [kb.sb("hyev%d" % j, [128, 512], es=es) for j in range(2)]
            evb = [kb.sb("hyevb%d" % j, [128, 512], BF16, es=es) for j in range(2)]
            wv = hy_w_in.ap().rearrange("(k p) n -> p k n", p=128)
            scnt = 0
            ecnt = 0
            for c in range(6):
                Wj = Wj2[c % 2]
                wq = c % 2
                cw = cw2[wq]
                cwk = "hycw%d" % wq
                for j in range(3):
                    kb.ld(cw[:, j, :], bc_row(hy_conv_w, j * 3 * D + c * 512, 512), w=[cwk])
                kb.ld(cw[:, 3, :], bc_row(hy_conv_b, c * 512, 512), w=[cwk])
                for pc in range(4):
                    sg_ = stg[scnt % 2]
                    sgk = "hystg%d" % (scnt % 2)
                    scnt += 1
                    kb.ld(sg_[:], wv[:, :, c * 512 + pc * 128:c * 512 + (pc + 1) * 128], w=[sgk])
                    for j in range(3):
                        kb.tt(("pool", "dve", "pool")[j], Wj[j][:, :, pc * 128:(pc + 1) * 128], sg_[:], bm(cw[:, j, pc * 128:(pc + 1) * 128], 8), ALU.mult,
                              r=[sgk, cwk], w=["hyW%d_%d" % (j, wq)])
                for t in range(NT):
                    pu, puk = kb.ps()
                    n = 0
                    for j in range(3):
                        for k in range(8):
                            kb.mm(pu[:], hT[:, k, t * 128 + j:t * 128 + j + 128], Wj[j][:, k, :], n == 0, n == 23, r=hkeys + ["hyW%d_%d" % (j, wq)], w=[puk])
                            n += 1
                    eb = ev[ecnt % 2]
                    ebk = "hyev%d" % (ecnt % 2)
                    kb.tt("dve", eb[:], pu[:], cw[:, 3, :], ALU.add, r=[puk, cwk], w=[ebk])
                    kb.ld(u_scr.ap()[t * 128:(t + 1) * 128, c * 512:(c + 1) * 512], eb[:], r=[ebk], w=["u_scr"])
                    if c < 2:
                        ebb = evb[ecnt % 2]
                        ebbk = "hyevb%d" % (ecnt % 2)
                        kb.cp("act", ebb[:], eb[:], r=[ebk], w=[ebbk])
                        kb.ld(vb_scr.ap()[t * 128:(t + 1) * 128, c * 512:(c + 1) * 512], ebb[:], r=[ebbk], w=["vb_scr"])
                    ecnt += 1
        kb.barrier()
        if stop == "hy2":
            return
        with ExitStack() as es:
            zT = kb.sb("hyzT", [33, L], es=es)
            w1 = kb.sb("hyw1", [33, 64], es=es)
            wh = kb.sb("hywh", [64, 2, 64], es=es)
            wo = kb.sb("hywo", [64, 4 * D], es=es)
            bT = kb.sb("hybT", [64, 3], es=es)
            sfT = kb.sb("hysfT", [64, 3], es=es)
            hid = [kb.sb("hyhid%d" % j, [64, L], es=es) for j in range(2)]
            msk = kb.sb("hymsk", [64, L], es=es)
            negpi = kb.sb("hynegpi", [64, 1], es=es)
            kb.ld(zT[:], hy_zT.ap(), w=["hyzT"])
            kb.ld(w1[:], hy_ffn_w_in.ap(), w=["hyw1"])
            kb.ld(wh[:], hy_ffn_w_hid.ap().rearrange("l i o -> i l o"), w=["hywh"])
            kb.ld(wo[:], hy_ffn_w_out.ap(), w=["hywo"])
            kb.ld(bT[:], hy_ffn_bT.ap(), w=["hybT"])
            kb.ld(sfT[:], hy_sin_freqT.ap(), w=["hysfT"])
            cur = None
            for l in range(3):
                dst = hid[l % 2]
                dk = "hyhid%d" % (l % 2)
                for q in range(4):
                    pp, ppk = kb.ps()
                    if l == 0:
                        kb.mm(pp[0:64, :], w1[:, :], zT[:, q * 512:(q + 1) * 512], True, True, r=["hyw1", "hyzT"], w=[ppk])
                    else:
                        kb.mm(pp[0:64, :], wh[:, l - 1, :], cur[:, q * 512:(q + 1) * 512], True, True, r=["hywh", curk], w=[ppk])
                    kb.ts("dve", dst[:, q * 512:(q + 1) * 512], pp[0:64, :], bT[:, l:l + 1], sfT[:, l:l + 1], ALU.add, ALU.mult, r=[ppk, "hybT", "hysfT"], w=[dk])
                for rep in range(2):
                    kb.ts("dve", msk[:], dst[:], -PI, 2 * PI, ALU.is_lt, ALU.mult, r=[dk], w=["hymsk"])
                    kb.ts("pool", dst[:], dst[:], PI, None, ALU.is_gt, None, r=[dk, "hymsk"], w=[dk + "g"]) if False else None
                    kb.stt("dve", msk[:], dst[:], PI, msk[:], ALU.is_gt, ALU.subtract, r=[dk, "hymsk"], w=["hymsk"]) if False else None
                    kb.tt("dve", dst[:], dst[:], msk[:], ALU.add, r=[dk, "hymsk"], w=[dk])
                    kb.ts("dve", msk[:], dst[:], PI, -2 * PI, ALU.is_gt, ALU.mult, r=[dk], w=["hymsk"])
                    kb.tt("dve", dst[:], dst[:], msk[:], ALU.add, r=[dk, "hymsk"], w=[dk])
                kb.act(dst[:], dst[:], AF.Sin, r=[dk], w=[dk])
                cur = dst
                curk = dk
            fd = kb.sb("hyfd", [128, 2 * D], es=es)
            bd = kb.sb("hybd", [128, 2 * D], es=es)
            sb_ = [kb.sb("hysb%d" % j, [128, 2 * D], BF16, es=es) for j in range(2)]
            dec = kb.sb("hydec", [128, D], es=es)
            for a in range(16):
                kb.ld(dec[:], hy_decay.ap()[a * 128:(a + 1) * 128, :], w=["hydec"])
                for q in range(8):
                    pf, pfk = kb.ps()
                    kb.mm(pf[:], cur[:, a * 128:(a + 1) * 128], wo[:, q * 512:(q + 1) * 512], True, True, r=[curk, "hywo"], w=[pfk])
                    half = q % 2
                    tgt = fd if q < 4 else bd
                    tk = "hyfd" if q < 4 else "hybd"
                    kb.tt("dve", tgt[:, (q % 4) * 512:(q % 4 + 1) * 512], pf[:], dec[:, half * 512:(half + 1) * 512], ALU.mult, r=[pfk, "hydec"], w=[tk])
                if a == 0:
                    kb.memset("dve", bd[0:1, :], 0.0, w=["hybd"])
                kb.tt("dve", sb_[0][:], fd[:], bd[:], ALU.add, r=["hyfd", "hybd"], w=["hysb0"])
                kb.tt("dve", sb_[1][:], fd[:], bd[:], ALU.subtract, r=["hyfd", "hybd"], w=["hysb1"])
                kb.ld(sd_scr.ap()[0, a * 128:(a + 1) * 128, :], sb_[0][:], r=["hysb0"], w=["sd_scr"])
                kb.ld(sd_scr.ap()[1, a * 128:(a + 1) * 128, :], sb_[1][:], r=["hysb1"], w=["sd_scr"])
        kb.barrier()
        with ExitStack() as es:
            sdt = kb.sb("hysdt", [128, 16, 512], BF16, es=es)
            ft = [kb.sb("hyft%d" % j, [128, 16, 128], BF16, es=es) for j in range(2)]
            he = [kb.sb("hyhe%d" % j, [128, 512], es=es) for j in range(2)]
            fcnt = 0
            for ri, dmat in enumerate((dft_c, dft_ms)):
                for cc in range(4):
                    kb.ld(sdt[:], sd_scr.ap()[ri].rearrange("(a p) n -> p a n", p=128)[:, :, cc * 512:(cc + 1) * 512], r=["sd_scr"], w=["hysdt"])
                    for kt in range(16):
                        f_ = ft[fcnt % 2]
                        fk = "hyft%d" % (fcnt % 2)
                        if fcnt == 0:
                            kb.ld(f_[:], dmat.ap()[kt], w=[fk])
                        nxt = fcnt + 1
                        if nxt < 128:
                            n_ri, n_kt = nxt // 64, nxt % 16
                            kb.ld(ft[nxt % 2][:], (dft_c, dft_ms)[n_ri].ap()[n_kt], w=["hyft%d" % (nxt % 2)])
                        ph, phk = kb.ps()
                        for a in range(16):
                            kb.mm(ph[:], f_[:, a, :], sdt[:, a, :], a == 0, a == 15, r=[fk, "hysdt"], w=[phk])
                        h_ = he[fcnt % 2]
                        hk = "hyhe%d" % (fcnt % 2)
                        fcnt += 1
                        kb.cp("act", h_[:], ph[:], r=[phk], w=[hk])
                        kb.ld(Hspec.ap()[ri, kt, :, cc * 512:(cc + 1) * 512], h_[:], r=[hk], w=["Hspec"])
        kb.barrier()
        with ExitStack() as es:
            z3T = kb.sb("hyz3T", [128, 8, L], BF16, es=es)
            zin = kb.sb("hyzin", [128, 16, 512], BF16, es=es)
            Pm = kb.sb("hyP", [128, 32, 512], BF16, es=es)
            ftcs = [kb.sb("hyftc%d" % j, [128, 16, 128], BF16, es=es) for j in range(2)]
            ftss = [kb.sb("hyfts%d" % j, [128, 16, 128], BF16, es=es) for j in range(2)]
            gts = [kb.sb("hygt%d" % j, [128, 32, 128], BF16, es=es) for j in range(2)]
            Ht = kb.sb("hyHt", [128, 2, 512], es=es)
            tmp = [kb.sb("hytmp%d" % j, [128, 512], es=es) for j in range(2)]
            tmp = [tmp[0], tmp[1], tmp[0], tmp[1]]
            dbc = 0
            gate = kb.sb("hygate", [128, 512], es=es)
            zp = kb.sb("hyzp", [128, 512], es=es)
            zo = kb.sb("hyzo", [128, 512], es=es)
            zob = kb.sb("hyzob", [128, 512], BF16, es=es)
            skb = kb.sb("hyskb", [128, 512], es=es)
            for o in range(2):
                src_b = vb_scr if o == 0 else z2b_scr
                src_f = u_scr if o == 0 else z2_scr
                for hf in range(2):
                    c0 = hf * 512
                    kb.ld(zin[:], src_b.ap().rearrange("(a p) n -> p a n", p=128)[:, :, c0:c0 + 512], r=["vb_scr", "z2b_scr"], w=["hyzin"])
                    kb.ld(skb[:], bc_row(hy_skip, o * D + c0, 512), w=["hyskb"])
                    kb.ld(ftcs[0][:], dft_c.ap()[0], w=["hyftc0"])
                    kb.ld(ftss[0][:], dft_ms.ap()[0], w=["hyfts0"])
                    for kt in range(16):
                        ftc = ftcs[kt % 2]
                        fts = ftss[kt % 2]
                        fck = "hyftc%d" % (kt % 2)
                        fsk = "hyfts%d" % (kt % 2)
                        if kt + 1 < 16:
                            kb.ld(ftcs[(kt + 1) % 2][:], dft_c.ap()[kt + 1], w=["hyftc%d" % ((kt + 1) % 2)])
                            kb.ld(ftss[(kt + 1) % 2][:], dft_ms.ap()[kt + 1], w=["hyfts%d" % ((kt + 1) % 2)])
                        kb.ld(Ht[:, 0, :], Hspec.ap()[0, kt, :, o * D + c0:o * D + c0 + 512], r=["Hspec"], w=["hyHt"])
                        kb.ld(Ht[:, 1, :], Hspec.ap()[1, kt, :, o * D + c0:o * D + c0 + 512], r=["Hspec"], w=["hyHt"])
                        pr, prk = kb.ps()
                        pi_, pik = kb.ps()
                        for a in range(16):
                            kb.mm(pr[:], ftc[:, a, :], zin[:, a, :], a == 0, a == 15, r=[fck, "hyzin"], w=[prk])
                        for a in range(16):
                            kb.mm(pi_[:], fts[:, a, :], zin[:, a, :], a == 0, a == 15, r=[fsk, "hyzin"], w=[pik])
                        kb.tt("dve", tmp[0][:], pr[:], Ht[:, 0, :], ALU.mult, r=[prk, "hyHt"], w=["hytmp0"])
                        kb.tt("dve", tmp[1][:], pi_[:], Ht[:, 1, :], ALU.mult, r=[pik, "hyHt"], w=["hytmp1"])
                        kb.tt("dve", Pm[:, kt, :], tmp[0][:], tmp[1][:], ALU.subtract, r=["hytmp0", "hytmp1"], w=["hyP%d" % kt])
                        kb.tt("dve", tmp[2][:], pr[:], Ht[:, 1, :], ALU.mult, r=[prk, "hyHt"], w=["hytmp0"])
                        kb.tt("dve", tmp[3][:], pi_[:], Ht[:, 0, :], ALU.mult, r=[pik, "hyHt"], w=["hytmp1"])
                        kb.tt("dve", Pm[:, 16 + kt, :], tmp[2][:], tmp[3][:], ALU.add, r=["hytmp0", "hytmp1"], w=["hyP%d" % (16 + kt)])
                    pkeys = ["hyP%d" % q for q in range(32)]
                    kb.ld(gts[0][:], idft.ap()[0], w=["hygt0"])
                    for t in range(NT):
                        gt = gts[t % 2]
                        gtk = "hygt%d" % (t % 2)
                        if t + 1 < NT:
                            kb.ld(gts[(t + 1) % 2][:], idft.ap()[t + 1], w=["hygt%d" % ((t + 1) % 2)])
                        py, pyk = kb.ps()
                        for f in range(32):
                            kb.mm(py[:], gt[:, f, :], Pm[:, f, :], f == 0, f == 31, r=[gtk] + pkeys, w=[pyk])
                        gcol = (1 + o) * D + c0
                        kb.ld(gate[:], u_scr.ap()[t * 128:(t + 1) * 128, gcol:gcol + 512], r=["u_scr"], w=["hygate"])
                        kb.ld(zp[:], src_f.ap()[t * 128:(t + 1) * 128, c0:c0 + 512], r=["u_scr", "z2_scr"], w=["hyzp"])
                        kb.tt("dve", zp[:], zp[:], skb[:], ALU.mult, r=["hyzp", "hyskb"], w=["hyzp"])
                        kb.tt("dve", zo[:], py[:], zp[:], ALU.add, r=[pyk, "hyzp"], w=["hyzo"])
                        kb.tt("dve", zo[:], zo[:], gate[:], ALU.mult, r=["hyzo", "hygate"], w=["hyzo"])
                        if o == 0:
                            kb.ld(z2_scr.ap()[t * 128:(t + 1) * 128, c0:c0 + 512], zo[:], r=["hyzo"], w=["z2_scr"])
                            kb.cp("act", zob[:], zo[:], r=["hyzo"], w=["hyzob"])
                            kb.ld(z2b_scr.ap()[t * 128:(t + 1) * 128, c0:c0 + 512], zob[:], r=["hyzob"], w=["z2b_scr"])
                        else:
                            pz, pzk = kb.ps()
                            for j in range(4):
                                kb.tr(pz[:, j * 128:(j + 1) * 128], zo[:, j * 128:(j + 1) * 128], ident_f[:], r=["hyzo"], w=[pzk])
                            kb.cp("act", z3T[:, hf * 4:(hf + 1) * 4, t * 128:(t + 1) * 128], pz[:].rearrange("p (j q) -> p j q", j=4), r=[pzk], w=["hyz3T%d" % t])
            kb.barrier()
            load_gate_ln(1, 0)
            wob = Pm[:, 0:16, :].rearrange("p a n -> p (a n)").rearrange("p (k n) -> p k n", k=8)
            wov = hy_w_out.ap().rearrange("(k p) n -> p k n", p=128)
            stgo = zin[:].bitcast(F32) if False else None
            for cc in range(8):
                kb.ld(Ht[:].rearrange("p a n -> p (a n)").rearrange("p (k n) -> p k n", k=8), wov[:, :, cc * 128:(cc + 1) * 128], w=["hyHt"])
                kb.cp("pool", wob[:, :, cc * 128:(cc + 1) * 128], Ht[:].rearrange("p a n -> p (a n)").rearrange("p (k n) -> p k n", k=8), r=["hyHt"], w=["hywob"])
            tmpo = zin.bitcast(F32)[:].rearrange("p a b -> p (a b)")[:, 0:D]
            stL = kb.sb("hystL", [128, 8], es=es)
            for t in range(NT):
                for c in range(2):
                    py, pyk = kb.ps()
                    for k in range(8):
                        kb.mm(py[:], z3T[:, k, t * 128:(t + 1) * 128], wob[:, k, c * 512:(c + 1) * 512], k == 0, k == 7, r=["hyz3T%d" % t, "hywob"], w=[pyk])
                    kb.tt("dve", tmpo[:, c * 512:(c + 1) * 512], py[:], g_bc[:, c * 512:(c + 1) * 512], ALU.mult, r=[pyk, "g_bc"], w=["tmp_o%d" % c])
                kb.stt("dve", x[:, t, :], x[:, t, :], ALPHA, tmpo[:], ALU.mult, ALU.add, r=["x%d" % t, "tmp_o0", "tmp_o1"], w=["x%d" % t, "tmp_o0", "tmp_o1"])
                ln_inplace(t, (tmpo, stL))
        kb.barrier()

    mod_phase(0, True)
    if stop == "mod":
        kb.ld(out_t.ap()[0:128, 0:48], cols[:].rearrange("p a b -> p (a b)"), r=["cols%d" % j for j in (0, 1, 3, 4)], w=["o"])
        kb.ld(out_t.ap()[128:256, 0:16], ccols[:].rearrange("p a b -> p (a b)"), r=["ccols0", "ccols1"], w=["o2"])
        kb.barrier()
        kb.emit()
        return nc, list(T.keys())
    attention_layer()
    kb.barrier()
    if stage >= 2:
        moe_layer(0)
    if stage >= 3:
        mod_phase(1, False)
        hyena_layer()
    if stage >= 4:
        moe_layer(1)

    for t in range(NT):
        kb.ld(out_t.ap()[t * 128:(t + 1) * 128, :], x[:, t, :], r=["x%d" % t], w=["out%d" % t])
    kb.barrier()
    print("ops", kb.n_ops, {e: len(kb.q[e]) for e in ENGS})
    kb.emit()
    return nc, list(T.keys())


def make_inputs(inputs, names, b):
    c = host_consts()
    m = {}
    for n in names:
        if n in ("modrow", "cmodrow", "u_scr", "vb_scr", "sd_scr", "Hspec", "z2_scr", "z2b_scr"):
            continue
        if n in c:
            m[n] = c[n]
        elif n == "x":
            m[n] = np.ascontiguousarray(inputs["x"][b])
        elif n == "ctx":
            m[n] = np.ascontiguousarray(inputs["ctx"][b])
        elif n == "cT":
            m[n] = np.ascontiguousarray(inputs["c"][b].reshape(8, 128).T)
        elif n == "cctxT":
            m[n] = np.ascontiguousarray(inputs["c_ctx"].reshape(8, 128).T)
        elif n == "attn_w_in":
            m[n] = inputs["attn_w_in"][0]
        elif n == "attn_lambda":
            m[n] = inputs["attn_lambda"][0].reshape(1, 256)
        elif n == "attn_subln_g":
            m[n] = inputs["attn_subln_g"][0].reshape(1, 128)
        elif n == "attn_sink":
            m[n] = inputs["attn_sink"][0].reshape(1, 8)
        elif n == "attn_w_out":
            m[n] = inputs["attn_w_out"][0]
        elif n == "router_bias":
            m[n] = inputs["router_bias"].reshape(1, NE)
        elif n in ("hy_w_in", "hy_conv_w", "hy_ffn_w_in", "hy_ffn_w_hid", "hy_ffn_w_out", "hy_skip", "hy_w_out"):
            m[n] = inputs[n][0]
        elif n == "hy_conv_b":
            m[n] = inputs[n][0].reshape(1, 3 * D)
        elif n == "hy_ffn_bT":
            m[n] = np.ascontiguousarray(inputs["hy_ffn_b"][0].T)
        elif n == "hy_sin_freqT":
            m[n] = np.ascontiguousarray(inputs["hy_sin_freq"][0].T)
        else:
            m[n] = inputs[n]
        m[n] = np.ascontiguousarray(m[n])
    return m


def kernel(**inputs):
    inputs = {k: np.asarray(v) for k, v in inputs.items()}
    nc, names = build_program()
    in_maps = [make_inputs(inputs, names, b) for b in range(8)]
    res = run_bass_kernel_spmd(nc, in_maps, core_ids=list(range(8)))
    out = np.stack([r["out"] for r in res.results], 0)
    return out.astype(np.float32)
```
